# Optimizing a Trainium2 kernel written in Bass

```python
import jax, jax.numpy as jnp
from jax import lax
import numpy as np

D_MODEL = 2048
BATCH = 8
SEQ = 2048
DEPTH = 1

HEAD_DIM_A = 64
N_Q_A = 16
N_KV_A = 4
WINDOW = 128
HEAD_DIM_B = 128
N_H_B = 8
MOBA_BLOCK = 256
MOBA_TOPK = 3
MOBA_Q_CHUNK = 16
D_FF = ((-((-8 * D_MODEL) // 3) + 255) // 256) * 256
RMS_EPS = 1e-6

WQ_A = N_Q_A * HEAD_DIM_A
WKV_A = N_KV_A * HEAD_DIM_A
W_B = N_H_B * HEAD_DIM_B
IN_COLS = WQ_A + 2 * WKV_A + 3 * W_B + 2 * D_MODEL

kernel_name = "hybrid_gated_swa_sink_moba_swiglu"


def rms_norm(x, g):
    xf = x.astype(jnp.float32)
    y = xf * lax.rsqrt(jnp.mean(xf * xf, axis=-1, keepdims=True) + RMS_EPS)
    return (y * g.astype(jnp.float32)).astype(x.dtype)


def alibi_slopes(n):
    return jnp.exp2(-8.0 * jnp.arange(1, n + 1, dtype=jnp.float32) / n)


def sliding_window_sink_attention(q, k, v, sinks, slopes):
    B, S, Hq, dh = q.shape
    Hkv = k.shape[2]
    R = Hq // Hkv
    L = WINDOW
    nb = S // L
    qb = (q * (dh ** -0.5)).reshape(B, nb, L, Hkv, R, dh)
    kb = k.reshape(B, nb, L, Hkv, dh)
    vb = v.reshape(B, nb, L, Hkv, dh)
    pad = ((0, 0), (1, 0), (0, 0), (0, 0), (0, 0))
    kwin = jnp.concatenate([jnp.pad(kb, pad)[:, :-1], kb], axis=2)
    vwin = jnp.concatenate([jnp.pad(vb, pad)[:, :-1], vb], axis=2)
    s = jnp.einsum('bnqgrd,bnkgd->bgrnqk', qb, kwin).astype(jnp.float32)
    qi = jnp.arange(L)[:, None]
    kj = jnp.arange(2 * L)[None, :]
    dist = L + qi - kj
    kpos = (jnp.arange(nb)[:, None, None] - 1) * L + kj[None]
    valid = (dist >= 0) & (dist < WINDOW) & (kpos >= 0)
    s = s - slopes.reshape(Hkv, R, 1, 1, 1) * dist.astype(jnp.float32)
    s = jnp.where(valid, s, -jnp.inf)
    sink = sinks.astype(jnp.float32).reshape(Hkv, R, 1, 1, 1)
    m = jnp.maximum(s.max(axis=-1, keepdims=True), sink)
    e = jnp.exp(s - m)
    p = e / (e.sum(axis=-1, keepdims=True) + jnp.exp(sink - m))
    o = jnp.einsum('bgrnqk,bnkgd->bnqgrd', p.astype(v.dtype), vwin)
    return o.reshape(B, S, Hq * dh)


def moba_attention(q, k, v, slopes):
    B, S, H, dh = q.shape
    nblk = -(-S // MOBA_BLOCK)
    Sp = nblk * MOBA_BLOCK
    padw = ((0, 0), (0, Sp - S), (0, 0), (0, 0))
    qh = jnp.pad(q * (dh ** -0.5), padw).transpose(0, 2, 1, 3)
    kh = jnp.pad(k, padw).transpose(0, 2, 1, 3)
    vh = jnp.pad(v, padw).transpose(0, 2, 1, 3)
    kblk = kh.reshape(B, H, nblk, MOBA_BLOCK, dh)
    vblk = vh.reshape(B, H, nblk, MOBA_BLOCK, dh)
    kmean = kblk.astype(jnp.float32).mean(axis=3)
    gate = jnp.einsum('bhsd,bhnd->bhsn', qh.astype(jnp.float32), kmean)
    cur = jnp.arange(Sp) // MOBA_BLOCK
    past = jnp.arange(nblk)[None, :] < cur[:, None]
    gate = jnp.where(past, gate, -jnp.inf)
    n_sel = min(MOBA_TOPK, nblk)
    _, sel = lax.top_k(gate, n_sel)
    sel_valid = sel < cur[:, None]
    bi = jnp.arange(B)[:, None, None, None]
    hi = jnp.arange(H)[None, :, None, None]
    slope_g = slopes[None, :, None, None, None]
    slope_o = slopes[None, :, None, None]
    offs = jnp.arange(MOBA_BLOCK)
    n_keys_sel = n_sel * MOBA_BLOCK

    def chunk(c):
        start = c * MOBA_Q_CHUNK
        qc = lax.dynamic_slice_in_dim(qh, start, MOBA_Q_CHUNK, axis=2)
        selc = lax.dynamic_slice_in_dim(sel, start, MOBA_Q_CHUNK, axis=2)
        valc = lax.dynamic_slice_in_dim(sel_valid, start, MOBA_Q_CHUNK, axis=2)
        own0 = (start // MOBA_BLOCK) * MOBA_BLOCK
        k_own = lax.dynamic_slice_in_dim(kh, own0, MOBA_BLOCK, axis=2)
        v_own = lax.dynamic_slice_in_dim(vh, own0, MOBA_BLOCK, axis=2)
        k_sel = kblk[bi, hi, selc]
        v_sel = vblk[bi, hi, selc]
        tq = start + jnp.arange(MOBA_Q_CHUNK)
        d_sel = (tq[:, None, None] - (selc[..., None] * MOBA_BLOCK + offs)).astype(jnp.float32)
        s_sel = jnp.einsum('bhqd,bhqjkd->bhqjk', qc, k_sel).astype(jnp.float32) - slope_g * d_sel
        s_sel = jnp.where(valc[..., None], s_sel, -jnp.inf)
        d_own = tq[:, None] - (own0 + offs)[None, :]
        s_own = jnp.einsum('bhqd,bhkd->bhqk', qc, k_own).astype(jnp.float32) - slope_o * d_own.astype(jnp.float32)
        s_own = jnp.where(d_own >= 0, s_own, -jnp.inf)
        logits = jnp.concatenate([s_sel.reshape(B, H, MOBA_Q_CHUNK, n_keys_sel), s_own], axis=-1)
        p = jax.nn.softmax(logits, axis=-1).astype(v.dtype)
        p_sel = p[..., :n_keys_sel].reshape(B, H, MOBA_Q_CHUNK, n_sel, MOBA_BLOCK)
        p_own = p[..., n_keys_sel:]
        return (jnp.einsum('bhqjk,bhqjkd->bhqd', p_sel, v_sel)
                + jnp.einsum('bhqk,bhkd->bhqd', p_own, v_own))

    o = lax.map(chunk, jnp.arange(Sp // MOBA_Q_CHUNK))
    o = o.transpose(1, 0, 3, 2, 4).reshape(B, Sp, H * dh)
    return o[:, :S]


def setup_inputs(seed: int = 0) -> dict:
    key = jax.random.key(seed)
    ks = jax.random.split(key, 16)
    f = jnp.float32

    def nrm(k, shape, scale):
        return jax.random.normal(k, shape, f) * scale

    return {
        "x": nrm(ks[0], (BATCH, SEQ, D_MODEL), 1.0),
        "norm1_g": 1.0 + nrm(ks[1], (DEPTH, D_MODEL), 0.02),
        "w_in": nrm(ks[2], (DEPTH, D_MODEL, IN_COLS), D_MODEL ** -0.5),
        "b_gate": nrm(ks[3], (DEPTH, 2 * D_MODEL), 0.02),
        "q_norm_a": 1.0 + nrm(ks[4], (DEPTH, HEAD_DIM_A), 0.02),
        "k_norm_a": 1.0 + nrm(ks[5], (DEPTH, HEAD_DIM_A), 0.02),
        "sinks_a": nrm(ks[6], (DEPTH, N_Q_A), 0.5),
        "q_norm_b": 1.0 + nrm(ks[7], (DEPTH, HEAD_DIM_B), 0.02),
        "k_norm_b": 1.0 + nrm(ks[8], (DEPTH, HEAD_DIM_B), 0.02),
        "w_branch_a": nrm(ks[9], (DEPTH, WQ_A, D_MODEL), WQ_A ** -0.5),
        "w_branch_b": nrm(ks[10], (DEPTH, W_B, D_MODEL), W_B ** -0.5),
        "w_o": nrm(ks[11], (DEPTH, D_MODEL, D_MODEL), D_MODEL ** -0.5),
        "norm2_g": 1.0 + nrm(ks[12], (DEPTH, D_MODEL), 0.02),
        "w_ffn_gate": nrm(ks[13], (DEPTH, D_MODEL, D_FF), D_MODEL ** -0.5),
        "w_ffn_up": nrm(ks[14], (DEPTH, D_MODEL, D_FF), D_MODEL ** -0.5),
        "w_ffn_down": nrm(ks[15], (DEPTH, D_FF, D_MODEL), D_FF ** -0.5),
    }


def reference(x, norm1_g, w_in, b_gate, q_norm_a, k_norm_a, sinks_a, q_norm_b, k_norm_b,
              w_branch_a, w_branch_b, w_o, norm2_g, w_ffn_gate, w_ffn_up, w_ffn_down):
    B, S, _ = x.shape
    slopes_a = alibi_slopes(N_Q_A)
    slopes_b = alibi_slopes(N_H_B)
    cuts = np.cumsum([WQ_A, WKV_A, WKV_A, W_B, W_B, W_B, D_MODEL]).tolist()
    h = x
    for l in range(DEPTH):
        u = rms_norm(h, norm1_g[l])
        z = u @ w_in[l]
        qa, ka, va, qb, kb, vb, ga, gb = jnp.split(z, cuts, axis=-1)
        qa = rms_norm(qa.reshape(B, S, N_Q_A, HEAD_DIM_A), q_norm_a[l])
        ka = rms_norm(ka.reshape(B, S, N_KV_A, HEAD_DIM_A), k_norm_a[l])
        va = va.reshape(B, S, N_KV_A, HEAD_DIM_A)
        o_a = sliding_window_sink_attention(qa, ka, va, sinks_a[l], slopes_a)
        qb = rms_norm(qb.reshape(B, S, N_H_B, HEAD_DIM_B), q_norm_b[l])
        kb = rms_norm(kb.reshape(B, S, N_H_B, HEAD_DIM_B), k_norm_b[l])
        vb = vb.reshape(B, S, N_H_B, HEAD_DIM_B)
        o_b = moba_attention(qb, kb, vb, slopes_b)
        gate_a = jax.nn.sigmoid(ga.astype(jnp.float32) + b_gate[l, :D_MODEL]).astype(h.dtype)
        gate_b = jax.nn.sigmoid(gb.astype(jnp.float32) + b_gate[l, D_MODEL:]).astype(h.dtype)
        mixed = gate_a * (o_a @ w_branch_a[l]) + gate_b * (o_b @ w_branch_b[l])
        h = h + mixed @ w_o[l]
        u2 = rms_norm(h, norm2_g[l])
        h = h + (jax.nn.silu(u2 @ w_ffn_gate[l]) * (u2 @ w_ffn_up[l])) @ w_ffn_down[l]
    return h
```

```python
import contextlib
import numpy as np
import ml_dtypes
import concourse.bass as bass
import concourse.mybir as mybir
from concourse.bass_utils import run_bass_kernel_spmd

F32 = mybir.dt.float32
BF16 = mybir.dt.bfloat16
AF = mybir.ActivationFunctionType
ALU = mybir.AluOpType
AX = mybir.AxisListType

S = 2048
D = 2048
NT = S // 128
KC = D // 128
DFF = 5632
FC = DFF // 128
EPS = 1e-6
NEG = -30000.0
WOC = 256
DEBUG = False


def slopes(n):
    return np.exp2(-8.0 * np.arange(1, n + 1, dtype=np.float64) / n)


class Buf:
    __slots__ = ("name", "w", "r")

    def __init__(self, name):
        self.name = name
        self.w = None
        self.r = {}


class Chan:
    def __init__(self, sem):
        self.sem = sem
        self.count = 0


ENGS = ["pe", "act", "dve", "pool", "sp"]


class Sched:
    def __init__(self, nc, stack):
        self.nc = nc
        self.stack = stack
        self.plan = {e: [] for e in ENGS}
        self.cnt = {e: 0 for e in ENGS}
        self.seen = {e: {} for e in ENGS}
        self.sem = {e: stack.enter_context(nc.semaphore("sem_" + e)) for e in ENGS}
        self.semobj = {}
        for e in ENGS:
            self.semobj["E" + e] = self.sem[e]
        self.chans = []

    def chan(self, name):
        sem = self.stack.enter_context(self.nc.semaphore("ch_" + name))
        c = Chan(sem)
        c.key = "C%d" % len(self.chans)
        self.semobj[c.key] = sem
        self.chans.append(c)
        return c

    def _deps(self, reads, writes):
        deps = {}

        def add(k, v):
            if deps.get(k, 0) < v:
                deps[k] = v
        for b in reads:
            if b.w is not None:
                add(*b.w)
        for b in writes:
            if b.w is not None:
                add(*b.w)
            for k, v in b.r.items():
                add(k, v)
        return deps

    def _waits(self, e, deps):
        for k, v in deps.items():
            if self.seen[e].get(k, 0) < v:
                self.plan[e].append(("wait", self.semobj[k], v))
                self.seen[e][k] = v

    def _commit(self, key, val, reads, writes):
        for b in reads:
            if b.r.get(key, 0) < val:
                b.r[key] = val
        for b in writes:
            b.w = (key, val)
            b.r = {}

    def op(self, e, fns, reads=(), writes=()):
        if not isinstance(fns, (list, tuple)):
            fns = [fns]
        self._waits(e, self._deps(reads, writes))
        self.cnt[e] += 1
        self.plan[e].append(("ops", list(fns), self.sem[e]))
        self._commit("E" + e, self.cnt[e], reads, writes)

    def dma(self, q, fn, ch, reads=(), writes=()):
        deps = self._deps(reads, writes)
        own = [b for b in writes if b.w is not None and b.w[0] == ch.key and not b.r]
        if own and all(b.w is None or b.w[0] == ch.key for b in writes):
            only_own = True
            for b in reads:
                if b.w is not None and b.w[0] == ch.key:
                    only_own = False
            if only_own:
                deps.pop(ch.key, None)
        self._waits(q, deps)
        ch.count += 16
        self.plan[q].append(("dma", fn, ch.sem))
        self._commit(ch.key, ch.count, reads, writes)

    def barrier(self):
        for e in ENGS:
            deps = {}
            for o in ENGS:
                if self.cnt[o] > 0:
                    deps["E" + o] = self.cnt[o]
            for c in self.chans:
                if c.count > 0:
                    deps[c.key] = c.count
            self._waits(e, deps)

    def replay(self, e, eng):
        for it in self.plan[e]:
            if it[0] == "wait":
                eng.wait_ge(it[1], it[2])
            elif it[0] == "ops":
                fns = it[1]
                for f in fns[:-1]:
                    f(eng)
                fns[-1](eng).then_inc(it[2], 1)
            else:
                it[1](eng).then_inc(it[2], 16)


def split_bf16(x, n):
    parts = []
    r = np.asarray(x, np.float64)
    for _ in range(n):
        p = r.astype(np.float32).astype(ml_dtypes.bfloat16)
        parts.append(p)
        r = r - p.astype(np.float64)
    return parts


def make_consts():
    bf = ml_dtypes.bfloat16
    c = {}
    c["ident"] = np.eye(128, dtype=np.float32).astype(bf)
    c["identf"] = np.eye(128, dtype=np.float32)
    j = np.arange(128)[:, None]
    cc = np.arange(256)[None, :]
    dist = cc - j
    c["maskA"] = np.where((dist >= 0) & (dist < 128), 0.0, NEG).astype(np.float32).astype(bf)
    mb = np.zeros((128, 512), np.float32)
    i = np.arange(128)[None, :]
    mb[:, :128] = np.where(j <= i, 0.0, NEG)
    c["maskB"] = mb.astype(bf)
    o3 = np.zeros((128, 128), np.float32)
    o3[0:3, :] = 1.0
    c["ones3"] = o3.astype(bf)
    sa = slopes(16)
    at = np.zeros((128, 16, 256), dtype=bf)
    for h in range(16):
        parts = split_bf16(-sa[h] * np.arange(256, dtype=np.float64), 3)
        for k in range(3):
            at[k, h, :] = parts[k]
    c["atab"] = at
    en = np.zeros((128, 8, 128), np.float32)
    for nb in range(8):
        en[nb, nb, :] = 1.0
        en[8 + nb, nb, :] = 1.0
    c["enb"] = en.astype(bf)
    bo = np.zeros((128, 128), np.float32)
    bo[:64, :64] = 1.0
    bo[64:, 64:] = 1.0
    c["blkones"] = bo.astype(bf)
    c["ones128"] = np.ones((128, 128), np.float32).astype(bf)
    lo = np.zeros((128, 128), np.float32)
    lo[:, :64] = 1.0
    hi = np.zeros((128, 128), np.float32)
    hi[:, 64:] = 1.0
    c["oneslo"] = lo.astype(bf)
    c["oneshi"] = hi.astype(bf)
    p = np.arange(128, dtype=np.float64)
    c["slpA"] = (p[:, None] * sa[None, :]).astype(np.float32)
    sb = slopes(8)
    slb = np.zeros((128, 8, 2), np.float64)
    for h in range(8):
        for half in range(2):
            slb[:, h, half] = sb[h] * (half * 128 + p)
    c["slpB"] = slb.astype(np.float32).reshape(128, 16)
    tq = np.zeros((128, 16, 8), np.float64)
    cm = np.zeros((128, 16, 8), np.float32)
    no = np.ones((128, 16, 8), np.float32)
    for n in range(16):
        for nb in range(8):
            tq[:, n, nb] = n * 128 + p - 256 * nb
            if nb >= n // 2:
                cm[:, n, nb] = -1e30
        no[:, n, n // 2] = 0.0
    c["tqrel"] = tq.astype(np.float32).reshape(128, 128)
    c["cmask"] = cm.reshape(128, 128)
    c["notown"] = no.reshape(128, 128)
    return c


CONST_SHAPES = None


def build_nc(consts):
    nc = bass.Bass("TRN2", target_bir_lowering=False)
    sA = slopes(16)
    sB = slopes(8)

    def din(name, shape, dt=F32):
        return nc.dram_tensor(name, list(shape), dt, kind="ExternalInput").ap()

    x = din("x", [S, D])
    win = din("win_t", [68, 128, 16 * 128])
    wba = din("wba_t", [16, 128, 8 * 128])
    wbb = din("wbb_t", [16, 128, 8 * 128])
    wo = din("wo_t", [D // WOC, 128, 16 * WOC])
    wg = din("wg_t", [FC, 128, 16 * 128])
    wu = din("wu_t", [FC, 128, 16 * 128])
    wd = din("wd_t", [16, 128, FC * 128])
    g1b_d = din("g1b", [128, D])
    g2b_d = din("g2b", [128, D])
    small_d = din("small", [128, 64])
    cd = {}
    for k, v in consts.items():
        cd[k] = din("c_" + k, v.shape, BF16 if v.dtype == ml_dtypes.bfloat16 else F32)
    out = nc.dram_tensor("out", [S, D], F32, kind="ExternalOutput").ap()
    dbg = {}
    if DEBUG:
        dbg["uT"] = nc.dram_tensor("dbg_uT", [128, KC * S], BF16, kind="ExternalOutput").ap()
        dbg["oaT"] = nc.dram_tensor("dbg_oaT", [128, 8 * S], BF16, kind="ExternalOutput").ap()
        dbg["obT"] = nc.dram_tensor("dbg_obT", [128, 8 * S], BF16, kind="ExternalOutput").ap()
        dbg["h1"] = nc.dram_tensor("dbg_h1", [S, D], F32, kind="ExternalOutput").ap()
        for nm in ("klo", "khi", "qTa", "vlo", "vhi"):
            dbg[nm] = nc.dram_tensor("dbg_" + nm, [128, S], BF16, kind="ExternalOutput").ap()
        for nm in ("pt0", "pt1"):
            dbg[nm] = nc.dram_tensor("dbg_" + nm, [128, 256], BF16, kind="ExternalOutput").ap()
        dbg["prm"] = nc.dram_tensor("dbg_prm", [128, 64], F32, kind="ExternalOutput").ap()

    with contextlib.ExitStack() as top:
        sc = Sched(nc, top)

        def sb(stack, name, shape, dt):
            t = stack.enter_context(nc.sbuf_tensor("sb_" + name, list(shape), dt))
            return t

        ps = [top.enter_context(nc.psum_tensor("ps%d" % i, [128, 512], F32)) for i in range(8)]
        PB = [Buf("ps%d" % i) for i in range(8)]

        UT = [Buf("uT%d" % t) for t in range(NT)]
        OA = [[Buf("oa%d_%d" % (c, q)) for q in range(4)] for c in range(8)]
        OB = [[Buf("ob%d_%d" % (h, q)) for q in range(4)] for h in range(8)]

        ct = {}
        PH_CONSTS = {"atab": 1, "maskA": 1, "enb": 2, "maskB": 2, "tqrel": 2, "cmask": 2, "notown": 2}

        def alloc_consts(stack, names):
            for k in names:
                v = consts[k]
                ct[k] = sb(stack, "k_" + k, v.shape, BF16 if v.dtype == ml_dtypes.bfloat16 else F32)

        def load_consts(names):
            for k in names:
                sc.dma("sp", (lambda k: lambda e: e.dma_start(out=ct[k][:], in_=cd[k]))(k), chc, writes=[CB])

        GLOB = [k for k in consts if k not in PH_CONSTS]
        alloc_consts(top, GLOB)
        small = sb(top, "small", [128, 64], F32)
        prm = sb(top, "prm", [128, 64], F32)
        CB = Buf("consts")
        PRM = Buf("prm")
        chc = sc.chan("const")

        gbro = sb(top, "gbro", [128, 384], F32)
        gbro_d = din("gbro", [128, 384])

        load_consts(GLOB)
        sc.dma("sp", lambda e: e.dma_start(out=small[:], in_=small_d), chc, writes=[CB])
        sc.dma("sp", lambda e: e.dma_start(out=gbro[:], in_=gbro_d), chc, writes=[CB])

        def dv(f, reads=(), writes=()):
            sc.op("dve", f, reads, writes)

        def ac(f, reads=(), writes=()):
            sc.op("act", f, reads, writes)

        for i, (lo_, hi_) in enumerate([(0, 64), (64, 128), (128, 256), (256, 384)]):
            dv((lambda i, lo_, hi_: lambda e: e.tensor_reduce(
                out=prm[:, 6 + i:7 + i], in_=gbro[:, lo_:hi_], axis=AX.X, op=ALU.max,
                apply_absolute_value=True))(i, lo_, hi_), reads=[CB], writes=[PRM])
        dv(lambda e: e.tensor_scalar(out=prm[:, 0:1], in0=small[:, 1:2], scalar1=8.0, scalar2=None, op0=ALU.mult),
           reads=[CB], writes=[PRM])
        dv(lambda e: e.tensor_scalar(out=prm[:, 1:2], in0=small[:, 3:4], scalar1=float(np.sqrt(128.0)),
                                     scalar2=None, op0=ALU.mult), reads=[CB], writes=[PRM])
        dv(lambda e: e.scalar_tensor_tensor(out=prm[:, 2:3], in0=prm[:, 6:7], scalar=8.0, in1=prm[:, 7:8],
                                            op0=ALU.mult, op1=ALU.mult), reads=[PRM], writes=[PRM])
        dv(lambda e: e.scalar_tensor_tensor(out=prm[:, 3:4], in0=prm[:, 8:9], scalar=float(np.sqrt(128.0)),
                                            in1=prm[:, 9:10], op0=ALU.mult, op1=ALU.mult), reads=[PRM], writes=[PRM])
        dv(lambda e: e.tensor_scalar(out=prm[:, 4:6], in0=prm[:, 2:4], scalar1=-1.0, scalar2=None, op0=ALU.mult),
           reads=[PRM], writes=[PRM])
        dv(lambda e: e.tensor_scalar(out=prm[:, 16:32], in0=ct["slpA"][:], scalar1=prm[:, 2:3], scalar2=None,
                                     op0=ALU.subtract), reads=[PRM, CB], writes=[PRM])
        dv(lambda e: e.tensor_scalar(out=prm[:, 32:48], in0=ct["slpB"][:], scalar1=prm[:, 3:4], scalar2=None,
                                     op0=ALU.subtract), reads=[PRM, CB], writes=[PRM])
        ac(lambda e: e.activation(out=prm[:, 48:56], in_=small[:, 4:12], func=AF.Exp, bias=prm[:, 4:5], scale=1.0),
           reads=[PRM, CB], writes=[PRM])

        def rmsnorm_to_T(stack, src_loader, gb_dram, dstT, DST, ntiles, tok0, tagp):
            xt = [sb(stack, tagp + "xt%d" % i, [128, D], F32) for i in range(2)]
            xn = [sb(stack, tagp + "xn%d" % i, [128, D], BF16) for i in range(2)]
            gbt = sb(stack, tagp + "gb", [128, D], F32)
            st = sb(stack, tagp + "st", [128, 3 * ntiles], F32)
            XT = [Buf(tagp + "xt%d" % i) for i in range(2)]
            XN = [Buf(tagp + "xn%d" % i) for i in range(2)]
            GB = Buf(tagp + "gb")
            ST = [Buf(tagp + "st%d" % i) for i in range(ntiles)]
            chx = [sc.chan(tagp + "x%d" % i) for i in range(2)]
            chg = sc.chan(tagp + "g")
            sc.dma("sp", lambda e: e.dma_start(out=gbt[:], in_=gb_dram), chg, writes=[GB])
            for t in range(ntiles):
                s_ = t % 2
                src, srcbufs = src_loader(t)
                sc.dma("sp", (lambda s_, src: lambda e: e.dma_start(out=xt[s_][:], in_=src))(s_, src),
                       chx[s_], reads=srcbufs, writes=[XT[s_]])
                ac((lambda s_, t: lambda e: e.activation(out=xn[s_][:], in_=xt[s_][:], func=AF.Square,
                                                         accum_out=st[:, 3 * t:3 * t + 1]))(s_, t),
                   reads=[XT[s_]], writes=[XN[s_], ST[t]])
                ac((lambda t: lambda e: e.activation(out=st[:, 3 * t + 1:3 * t + 2], in_=st[:, 3 * t:3 * t + 1],
                                                     func=AF.Ln, bias=ct_eps[:, 0:1], scale=1.0 / D))(t),
                   reads=[ST[t], PRM], writes=[ST[t]])
                ac((lambda t: lambda e: e.activation(out=st[:, 3 * t + 2:3 * t + 3], in_=st[:, 3 * t + 1:3 * t + 2],
                                                     func=AF.Exp, scale=-0.5))(t),
                   reads=[ST[t]], writes=[ST[t]])
                dv((lambda s_, t: lambda e: e.scalar_tensor_tensor(
                    out=xn[s_][:], in0=xt[s_][:], scalar=st[:, 3 * t + 2:3 * t + 3], in1=gbt[:],
                    op0=ALU.mult, op1=ALU.mult))(s_, t),
                   reads=[XT[s_], ST[t], GB], writes=[XN[s_]])
                for half in range(2):
                    bk = 4 + (2 * t + half) % 4
                    pbf = ps[bk][:].bitcast(BF16)
                    fns = []
                    for jj in range(8):
                        c = half * 8 + jj
                        fns.append((lambda s_, c, jj, pbf: lambda e: e.transpose(
                            out=pbf[:, jj * 128:(jj + 1) * 128], in_=xn[s_][:, c * 128:(c + 1) * 128],
                            identity=ct["ident"][:]))(s_, c, jj, pbf))
                    sc.op("pe", fns, reads=[XN[s_], CB], writes=[PB[bk]])
                    dstv = dstT[:, half * 8:(half + 1) * 8, tok0 + t * 128: tok0 + (t + 1) * 128]
                    srcv = pbf[:, 0:1024].rearrange("p (c t) -> p c t", c=8)
                    if half == 0:
                        ac((lambda dstv, srcv: lambda e: e.activation(out=dstv, in_=srcv, func=AF.Copy))(dstv, srcv),
                           reads=[PB[bk]], writes=[DST[t]])
                    else:
                        dv((lambda dstv, srcv: lambda e: e.tensor_copy(out=dstv, in_=srcv))(dstv, srcv),
                           reads=[PB[bk]], writes=[DST[t]])

        ct_eps = sb(top, "ct_eps", [128, 4], F32)
        sc.op("pool", lambda e: e.memset(ct_eps[:, 0:1], EPS), writes=[PRM])
        sc.op("pool", lambda e: e.memset(ct_eps[:, 1:2], EPS * 64), writes=[PRM])
        sc.op("pool", lambda e: e.memset(ct_eps[:, 2:3], EPS * 128), writes=[PRM])

        pers = contextlib.ExitStack()
        uT = sb(pers, "uT", [128, KC, S], BF16)
        oaT = sb(pers, "oaT", [128, 8, S], BF16)
        obT = sb(pers, "obT", [128, 8, S], BF16)

        with contextlib.ExitStack() as ph:
            rmsnorm_to_T(ph, lambda t: (x[t * 128:(t + 1) * 128, :], []), g1b_d, uT, UT, NT, 0, "p0")
            sc.barrier()

        if DEBUG:
            chd = sc.chan("dbg")
            sc.dma("sp", lambda e: e.dma_start(out=dbg["uT"], in_=uT[:].rearrange("p c t -> p (c t)")), chd,
                   reads=UT)

        def tg_bufs(tg):
            return UT[tg * 4:(tg + 1) * 4]

        def qknorm(psA_i, psB_i, ones_t, epscol, sq, SQ, lnv, LNV, gcol_ap, outs):
            ac(lambda e: e.activation(out=sq[:], in_=ps[psA_i][:], func=AF.Square),
               reads=[PB[psA_i]], writes=[SQ])
            sc.op("pe", lambda e: e.matmul(ps[psB_i][:], lhsT=ones_t[:], rhs=sq[:], start=True, stop=True),
                  reads=[SQ, CB], writes=[PB[psB_i]])
            ac(lambda e: e.activation(out=lnv[:], in_=ps[psB_i][:], func=AF.Ln, bias=ct_eps[:, epscol:epscol + 1],
                                      scale=1.0), reads=[PB[psB_i], PRM], writes=[LNV])
            ac(lambda e: e.activation(out=lnv[:], in_=lnv[:], func=AF.Exp, scale=-0.5), reads=[LNV], writes=[LNV])
            for (r0, r1, dst, dbufs, c0, c1, acc) in outs:
                def f(e, r0=r0, r1=r1, dst=dst, c0=c0, c1=c1, acc=acc):
                    kw = {}
                    if acc is not None:
                        kw["accum_out"] = acc
                    return e.scalar_tensor_tensor(out=dst, in0=ps[psA_i][r0:r1, c0:c1], scalar=gcol_ap[r0:r1, :],
                                                  in1=lnv[r0:r1, c0:c1], op0=ALU.mult, op1=ALU.mult, **kw)
                dv(f, reads=[PB[psA_i], LNV, PRM, CB], writes=dbufs)

        def load_w(stack_tile, src_ap, ch, WB, q="pool"):
            sc.dma(q, lambda e: e.dma_start(out=stack_tile, in_=src_ap), ch, writes=[WB])

        with contextlib.ExitStack() as ph:
            alloc_consts(ph, [k for k in PH_CONSTS if PH_CONSTS[k] == 1])
            load_consts([k for k in PH_CONSTS if PH_CONSTS[k] == 1])
            wk2 = [sb(ph, "a_wk%d" % i, [128, 16, 128], BF16) for i in range(2)]
            wv = [sb(ph, "a_wv%d" % i, [128, 16, 64], BF16) for i in range(2)]
            wq = [sb(ph, "a_wq%d" % i, [128, 16, 128], BF16) for i in range(2)]
            WKV = [Buf("a_wkv%d" % i) for i in range(2)]
            WQ = [Buf("a_wq%d" % i) for i in range(2)]
            chkv = [sc.chan("a_kv%d" % i) for i in range(2)]
            chq = [sc.chan("a_q%d" % i) for i in range(2)]
            Klo = sb(ph, "a_klo", [128, S], BF16)
            Khi = sb(ph, "a_khi", [128, S], BF16)
            vlo = sb(ph, "a_vlo", [128, NT, 128], BF16)
            vhi = sb(ph, "a_vhi", [128, NT, 128], BF16)
            qTa_ = sb(ph, "a_qT", [128, S], BF16)
            KB = [Buf("a_K%d" % i) for i in range(4)]
            VB = [Buf("a_V%d" % i) for i in range(2)]
            QB = [Buf("a_Q%d" % i) for i in range(4)]
            sq = [sb(ph, "a_sq%d" % i, [128, 512], BF16) for i in range(2)]
            lnv = [sb(ph, "a_ln%d" % i, [128, 512], F32) for i in range(2)]
            SQ = [Buf("a_sq%d" % i) for i in range(2)]
            LNV = [Buf("a_ln%d" % i) for i in range(2)]
            PT = [[sb(ph, "a_pt%d_%d" % (e_, i), [128, 256], BF16) for i in range(3)] for e_ in range(2)]
            PTB_ = [[Buf("a_pt%d_%d" % (e_, i)) for i in range(3)] for e_ in range(2)]
            den = sb(ph, "a_den", [128, 512], F32)
            DEN = Buf("a_den")

            sc.op("pool", lambda e: e.memset(Klo[64:128, :], 0.0), writes=KB)
            sc.op("pool", lambda e: e.memset(Khi[0:64, :], 0.0), writes=KB)
            sc.op("pool", lambda e: e.memset(vlo[:].rearrange("p t d -> p (t d)"), 0.0), writes=VB)
            sc.op("pool", lambda e: e.memset(vhi[:].rearrange("p t d -> p (t d)"), 0.0), writes=VB)

            rot = [0]

            def nbank():
                rot[0] = (rot[0] + 1) % 4
                return 4 + rot[0]

            def load_group(g):
                s_ = g % 2
                ck = 8 + g // 2
                cv = 10 + g // 2
                c0 = (g % 2) * 64
                srck = win[ck].rearrange("p (k n) -> p k n", k=16)[:, :, c0:c0 + 64]
                srcv = win[cv].rearrange("p (k n) -> p k n", k=16)[:, :, c0:c0 + 64]
                sc.dma("pool", lambda e: e.dma_start(out=wk2[s_][:, :, 0:64], in_=srck), chkv[s_], writes=[WKV[s_]])
                sc.dma("pool", lambda e: e.dma_start(out=wk2[s_][:, :, 64:128], in_=srck), chkv[s_], writes=[WKV[s_]])
                sc.dma("pool", lambda e: e.dma_start(out=wv[s_][:], in_=srcv), chkv[s_], writes=[WKV[s_]])

            def load_pair(c):
                s_ = c % 2
                sc.dma("pool", lambda e: e.dma_start(out=wq[s_][:].rearrange("p k n -> p (k n)"), in_=win[c]),
                       chq[s_], writes=[WQ[s_]])

            load_group(0)
            load_pair(0)
            for g in range(4):
                s_ = g % 2
                if g + 1 < 4:
                    load_group(g + 1)
                for tg in range(4):
                    bA = nbank()
                    bB = nbank()
                    fns = [(lambda kc, tg, bA, s_: lambda e: e.matmul(
                        ps[bA][:], lhsT=wk2[s_][:, kc, :], rhs=uT[:, kc, tg * 512:(tg + 1) * 512],
                        start=(kc == 0), stop=(kc == KC - 1)))(kc, tg, bA, s_) for kc in range(KC)]
                    sc.op("pe", fns, reads=[WKV[s_]] + tg_bufs(tg), writes=[PB[bA]])
                    i2 = tg % 2
                    qknorm(bA, bB, ct["blkones"], 1, sq[i2], SQ[i2], lnv[i2], LNV[i2], prm[:, 0:1],
                           [(0, 64, Klo[0:64, tg * 512:(tg + 1) * 512], [KB[tg]], 0, 512, None),
                            (64, 128, Khi[64:128, tg * 512:(tg + 1) * 512], [KB[tg]], 0, 512, None)])
                for half in range(2):
                    bV = nbank()
                    fns = []
                    for t8 in range(8):
                        t = half * 8 + t8
                        for kc in range(KC):
                            fns.append((lambda t, t8, kc, bV, s_: lambda e: e.matmul(
                                ps[bV][:, t8 * 64:(t8 + 1) * 64], lhsT=uT[:, kc, t * 128:(t + 1) * 128],
                                rhs=wv[s_][:, kc, :], start=(kc == 0), stop=(kc == KC - 1)))(t, t8, kc, bV, s_))
                    sc.op("pe", fns, reads=[WKV[s_]] + UT[half * 8:(half + 1) * 8], writes=[PB[bV]])
                    srcv = ps[bV][:].rearrange("p (t d) -> p t d", t=8)
                    ac((lambda half, srcv: lambda e: e.activation(
                        out=vlo[:, half * 8:(half + 1) * 8, 0:64], in_=srcv, func=AF.Copy))(half, srcv),
                       reads=[PB[bV]], writes=[VB[half]])
                    dv((lambda half, srcv: lambda e: e.tensor_copy(
                        out=vhi[:, half * 8:(half + 1) * 8, 64:128], in_=srcv))(half, srcv),
                       reads=[PB[bV]], writes=[VB[half]])
                for pp in range(2):
                    c = 2 * g + pp
                    sq_ = c % 2
                    if c + 1 < 8:
                        load_pair(c + 1)
                    for tg in range(4):
                        bA = nbank()
                        bB = nbank()
                        fns = [(lambda kc, tg, bA, sq_: lambda e: e.matmul(
                            ps[bA][:], lhsT=wq[sq_][:, kc, :], rhs=uT[:, kc, tg * 512:(tg + 1) * 512],
                            start=(kc == 0), stop=(kc == KC - 1)))(kc, tg, bA, sq_) for kc in range(KC)]
                        sc.op("pe", fns, reads=[WQ[sq_]] + tg_bufs(tg), writes=[PB[bA]])
                        i2 = tg % 2
                        qknorm(bA, bB, ct["blkones"], 1, sq[i2], SQ[i2], lnv[i2], LNV[i2], small[:, 0:1],
                               [(0, 128, qTa_[:, tg * 512:(tg + 1) * 512], [QB[tg]], 0, 512, None)])
                    sbank = {}

                    def a_S(m, c=c):
                        ncols = 256 if m < NT - 1 else 128
                        for e_ in range(2):
                            h = 2 * c + e_
                            bS = nbank()
                            sbank[(m, e_)] = bS
                            Kt = Klo if e_ == 0 else Khi
                            fns = [
                                (lambda Kt, m, ncols, bS: lambda e: e.matmul(
                                    ps[bS][:, :ncols], lhsT=Kt[:, m * 128:(m + 1) * 128],
                                    rhs=qTa_[:, m * 128:m * 128 + ncols], start=True, stop=False))(Kt, m, ncols, bS),
                                (lambda h, ncols, bS: lambda e: e.matmul(
                                    ps[bS][:, :ncols], lhsT=ct["ones3"][:], rhs=ct["atab"][:, h, :ncols],
                                    start=False, stop=False))(h, ncols, bS),
                                (lambda ncols, bS: lambda e: e.matmul(
                                    ps[bS][:, :ncols], lhsT=ct["ident"][:], rhs=ct["maskA"][:, :ncols],
                                    start=False, stop=True))(ncols, bS),
                            ]
                            qbs = [QB[m // 4]] + ([QB[(m + 1) // 4]] if m < NT - 1 else [])
                            sc.op("pe", fns, reads=[KB[m // 4], CB] + qbs, writes=[PB[bS]])

                    def a_E(m, c=c):
                        ncols = 256 if m < NT - 1 else 128
                        for e_ in range(2):
                            h = 2 * c + e_
                            bS = sbank[(m, e_)]
                            ac((lambda e_, m, ncols, bS, h: lambda e: e.activation(
                                out=PT[e_][m % 3][:, :ncols], in_=ps[bS][:, :ncols], func=AF.Exp,
                                bias=prm[:, 16 + h:17 + h], scale=1.0))(e_, m, ncols, bS, h),
                               reads=[PB[bS], PRM], writes=[PTB_[e_][m % 3]])

                    def a_PV(m, c=c):
                        qg = m // 4
                        bO = 2 * (qg % 2)
                        bD = bO + 1
                        for (bank, Lo, Hi, isv) in ((bO, vlo, vhi, True), (bD, ct["oneslo"], ct["oneshi"], False)):
                            fns = []
                            seq = []
                            for e_ in range(2):
                                W = Lo if e_ == 0 else Hi
                                if m > 0:
                                    seq.append((W, m - 1, PT[e_][(m - 1) % 3][:, 128:256]))
                                seq.append((W, m, PT[e_][m % 3][:, 0:128]))
                            for i_, (W, kt, rhs) in enumerate(seq):
                                lhs = W[:, kt, :] if isv else W[:]
                                fns.append((lambda lhs, rhs, i_, bank, m, n_=len(seq): lambda e: e.matmul(
                                    ps[bank][:, (m % 4) * 128:(m % 4 + 1) * 128], lhsT=lhs, rhs=rhs,
                                    start=(i_ == 0), stop=(i_ == n_ - 1)))(lhs, rhs, i_, bank, m))
                            rds = [PTB_[0][m % 3], PTB_[1][m % 3], VB[m // 8], CB]
                            if m > 0:
                                rds += [PTB_[0][(m - 1) % 3], PTB_[1][(m - 1) % 3], VB[(m - 1) // 8]]
                            sc.op("pe", fns, reads=rds, writes=[PB[bank]])
                        if m % 4 == 3:
                            dv((lambda bD, c: lambda e: e.tensor_scalar(
                                out=den[:], in0=ps[bD][:], scalar1=prm[:, 48 + c:49 + c], scalar2=None,
                                op0=ALU.add))(bD, c), reads=[PB[bD], PRM], writes=[DEN])
                            dv(lambda e: e.reciprocal(out=den[:], in_=den[:]), reads=[DEN], writes=[DEN])
                            dv((lambda bO, c, qg: lambda e: e.tensor_tensor(
                                out=oaT[:, c, qg * 512:(qg + 1) * 512], in0=ps[bO][:], in1=den[:],
                                op=ALU.mult))(bO, c, qg), reads=[PB[bO], DEN], writes=[OA[c][qg]])

                    a_S(0)
                    for m in range(NT):
                        if m + 1 < NT:
                            a_S(m + 1)
                        a_E(m)
                        a_PV(m)
            if DEBUG:
                chd2 = sc.chan("dbg2")
                for nm, t_, bufs in (("klo", Klo[:], KB), ("khi", Khi[:], KB), ("qTa", qTa_[:], QB),
                                     ("vlo", vlo[:].rearrange("p t d -> p (t d)"), VB),
                                     ("vhi", vhi[:].rearrange("p t d -> p (t d)"), VB)):
                    sc.dma("sp", (lambda nm, t_: lambda e: e.dma_start(out=dbg[nm], in_=t_))(nm, t_), chd2, reads=bufs)
                sc.dma("sp", lambda e: e.dma_start(out=dbg["pt0"], in_=PT[0][2][:]), chd2, reads=[PTB_[0][2]])
                sc.dma("sp", lambda e: e.dma_start(out=dbg["pt1"], in_=PT[1][2][:]), chd2, reads=[PTB_[1][2]])
                sc.dma("sp", lambda e: e.dma_start(out=dbg["prm"], in_=prm[:]), chd2, reads=[PRM])
            sc.barrier()

        with contextlib.ExitStack() as ph:
            alloc_consts(ph, [k for k in PH_CONSTS if PH_CONSTS[k] == 2])
            load_consts([k for k in PH_CONSTS if PH_CONSTS[k] == 2])
            wqb = [sb(ph, "b_wq%d" % i, [128, 16, 128], BF16) for i in range(2)]
            wkb = [sb(ph, "b_wk%d" % i, [128, 16, 128], BF16) for i in range(2)]
            wvb = [sb(ph, "b_wv%d" % i, [128, 16, 128], BF16) for i in range(2)]
            WBB = [Buf("b_w%d" % i) for i in range(2)]
            chw = [sc.chan("b_w%d" % i) for i in range(2)]
            kT = sb(ph, "b_kT", [128, S], BF16)
            qT = sb(ph, "b_qT", [128, S], BF16)
            vB = sb(ph, "b_v", [128, NT, 128], BF16)
            augb = sb(ph, "b_aug", [128, S], BF16)
            KB = [Buf("b_K%d" % i) for i in range(4)]
            QB = [Buf("b_Q%d" % i) for i in range(4)]
            VB = [Buf("b_V%d" % i) for i in range(4)]
            AG = Buf("b_aug")
            sq = [sb(ph, "b_sq%d" % i, [128, 512], BF16) for i in range(2)]
            lnv = [sb(ph, "b_ln%d" % i, [128, 512], F32) for i in range(2)]
            SQ = [Buf("b_sq%d" % i) for i in range(2)]
            LNV = [Buf("b_ln%d" % i) for i in range(2)]
            ksum = sb(ph, "b_ksum", [128, 8], F32)
            kmh = sb(ph, "b_kmh", [128, 8], BF16)
            kml = sb(ph, "b_kml", [128, 8], BF16)
            KS = Buf("b_ks")
            gm = sb(ph, "b_gm", [128, 128], F32)
            top8 = sb(ph, "b_top8", [128, 128], F32)
            gb_ = sb(ph, "b_gb", [128, 128], F32)
            tqs = sb(ph, "b_tqs", [128, 128], F32)
            comb = sb(ph, "b_comb", [128, 128], F32)
            cmb16 = sb(ph, "b_c16", [128, 16, 16], BF16)
            GT = Buf("b_gate")
            PTb = [sb(ph, "b_pt%d" % i, [128, 512], BF16) for i in range(3)]
            PTB_ = [Buf("b_pt%d" % i) for i in range(3)]
            rec = sb(ph, "b_rec", [128, 512], F32)
            REC = Buf("b_rec")
            sc.op("pool", lambda e: e.memset(augb[:], 0.0), writes=[AG])

            rot = [0]

            def nbank():
                rot[0] = (rot[0] + 1) % 4
                return 4 + rot[0]

            def load_head(h):
                s_ = h % 2
                for (t_, ci) in ((wqb, 12 + h), (wkb, 20 + h), (wvb, 28 + h)):
                    sc.dma("pool", (lambda t_, ci, s_: lambda e: e.dma_start(
                        out=t_[s_][:].rearrange("p k n -> p (k n)"), in_=win[ci]))(t_, ci, s_),
                        chw[s_], writes=[WBB[s_]])

            load_head(0)
            ptc = [0]
            for h in range(8):
                s_ = h % 2
                if h + 1 < 8:
                    load_head(h + 1)
                for tg in range(4):
                    bA = nbank()
                    bB = nbank()
                    fns = [(lambda kc, tg, bA, s_: lambda e: e.matmul(
                        ps[bA][:], lhsT=wkb[s_][:, kc, :], rhs=uT[:, kc, tg * 512:(tg + 1) * 512],
                        start=(kc == 0), stop=(kc == KC - 1)))(kc, tg, bA, s_) for kc in range(KC)]
                    sc.op("pe", fns, reads=[WBB[s_]] + tg_bufs(tg), writes=[PB[bA]])
                    i2 = tg % 2
                    qknorm(bA, bB, ct["ones128"], 2, sq[i2], SQ[i2], lnv[i2], LNV[i2], prm[:, 1:2],
                           [(0, 128, kT[:, tg * 512 + hh * 256: tg * 512 + (hh + 1) * 256], [KB[tg], KS],
                             hh * 256, (hh + 1) * 256, ksum[:, 2 * tg + hh: 2 * tg + hh + 1]) for hh in range(2)])
                dv(lambda e: e.tensor_scalar(out=kmh[:], in0=ksum[:], scalar1=1.0 / 256, scalar2=None, op0=ALU.mult),
                   reads=[KS], writes=[KS])
                dv(lambda e: e.scalar_tensor_tensor(out=kml[:], in0=ksum[:], scalar=1.0 / 256, in1=kmh[:],
                                                    op0=ALU.mult, op1=ALU.subtract), reads=[KS], writes=[KS])
                for tg in range(4):
                    bA = nbank()
                    bB = nbank()
                    fns = [(lambda kc, tg, bA, s_: lambda e: e.matmul(
                        ps[bA][:], lhsT=wqb[s_][:, kc, :], rhs=uT[:, kc, tg * 512:(tg + 1) * 512],
                        start=(kc == 0), stop=(kc == KC - 1)))(kc, tg, bA, s_) for kc in range(KC)]
                    sc.op("pe", fns, reads=[WBB[s_]] + tg_bufs(tg), writes=[PB[bA]])
                    i2 = tg % 2
                    qknorm(bA, bB, ct["ones128"], 2, sq[i2], SQ[i2], lnv[i2], LNV[i2], small[:, 2:3],
                           [(0, 128, qT[:, tg * 512:(tg + 1) * 512], [QB[tg]], 0, 512, None)])
                bG = nbank()
                fns = []
                for n in range(NT):
                    for ii, km in enumerate((kmh, kml)):
                        fns.append((lambda n, ii, km, bG: lambda e: e.matmul(
                            ps[bG][:, n * 8:(n + 1) * 8], lhsT=qT[:, n * 128:(n + 1) * 128], rhs=km[:],
                            start=(ii == 0), stop=(ii == 1)))(n, ii, km, bG))
                sc.op("pe", fns, reads=QB + [KS], writes=[PB[bG]])
                dv((lambda bG: lambda e: e.tensor_tensor(out=gm[:], in0=ps[bG][:, 0:128], in1=ct["cmask"][:],
                                                         op=ALU.add))(bG), reads=[PB[bG], CB], writes=[GT])
                for n in range(NT):
                    dv((lambda n: lambda e: e.max(out=top8[:, n * 8:(n + 1) * 8], in_=gm[:, n * 8:(n + 1) * 8]))(n),
                       reads=[GT], writes=[GT])
                for n in range(NT):
                    dv((lambda n: lambda e: e.tensor_scalar(
                        out=gb_[:, n * 8:(n + 1) * 8], in0=gm[:, n * 8:(n + 1) * 8],
                        scalar1=top8[:, n * 8 + 2:n * 8 + 3], scalar2=1.0, op0=ALU.is_ge, op1=ALU.subtract))(n),
                       reads=[GT], writes=[GT])
                dv(lambda e: e.tensor_tensor(out=gb_[:], in0=gb_[:], in1=ct["notown"][:], op=ALU.mult),
                   reads=[GT, CB], writes=[GT])
                dv((lambda h: lambda e: e.tensor_scalar(out=tqs[:], in0=ct["tqrel"][:], scalar1=float(-sB[h]),
                                                        scalar2=None, op0=ALU.mult))(h), reads=[CB, GT], writes=[GT])
                dv(lambda e: e.scalar_tensor_tensor(out=comb[:], in0=gb_[:], scalar=-NEG, in1=tqs[:],
                                                    op0=ALU.mult, op1=ALU.add), reads=[GT], writes=[GT])
                c3 = comb[:].rearrange("p (n b) -> p n b", n=16)
                dv(lambda e: e.tensor_copy(out=cmb16[:, :, 0:8], in_=c3), reads=[GT], writes=[GT])
                dv(lambda e: e.tensor_tensor(out=cmb16[:, :, 8:16], in0=c3, in1=cmb16[:, :, 0:8], op=ALU.subtract),
                   reads=[GT], writes=[GT])
                for qd in range(4):
                    bV = nbank()
                    fns = []
                    for t4 in range(4):
                        t = qd * 4 + t4
                        for kc in range(KC):
                            fns.append((lambda t, t4, kc, bV, s_: lambda e: e.matmul(
                                ps[bV][:, t4 * 128:(t4 + 1) * 128], lhsT=uT[:, kc, t * 128:(t + 1) * 128],
                                rhs=wvb[s_][:, kc, :], start=(kc == 0), stop=(kc == KC - 1)))(t, t4, kc, bV, s_))
                    sc.op("pe", fns, reads=[WBB[s_]] + UT[qd * 4:(qd + 1) * 4], writes=[PB[bV]])
                    srcv = ps[bV][:].rearrange("p (t d) -> p t d", t=4)
                    if qd % 2 == 0:
                        ac((lambda qd, srcv: lambda e: e.activation(
                            out=vB[:, qd * 4:(qd + 1) * 4, :], in_=srcv, func=AF.Copy))(qd, srcv),
                           reads=[PB[bV]], writes=[VB[qd]])
                    else:
                        dv((lambda qd, srcv: lambda e: e.tensor_copy(
                            out=vB[:, qd * 4:(qd + 1) * 4, :], in_=srcv))(qd, srcv),
                           reads=[PB[bV]], writes=[VB[qd]])
                for half in range(2):
                    bT = nbank()
                    pbf = ps[bT][:].bitcast(BF16)
                    fns = []
                    for jj in range(8):
                        n = half * 8 + jj
                        fns.append((lambda n, jj, pbf: lambda e: e.transpose(
                            out=pbf[0:16, jj * 128:(jj + 1) * 128], in_=cmb16[:, n, :],
                            identity=ct["ident"][:]))(n, jj, pbf))
                    sc.op("pe", fns, reads=[GT, CB], writes=[PB[bT]])
                    dv((lambda half, pbf: lambda e: e.tensor_copy(
                        out=augb[0:16, half * 1024:(half + 1) * 1024], in_=pbf[0:16, 0:1024]))(half, pbf),
                       reads=[PB[bT]], writes=[AG])
                steps = [(Q, kt) for Q in range(4) for kt in range(4 * Q + 4)]
                sbank = {}

                def geo(Q, kt):
                    q0 = max(kt * 128, Q * 512)
                    return q0, (Q + 1) * 512 - q0, q0 - Q * 512, kt * 128 >= Q * 512

                def b_S(i):
                    Q, kt = steps[i]
                    nb = kt // 2
                    q0, ncols, off, diag = geo(Q, kt)
                    bS = nbank()
                    sbank[i] = bS
                    fns = [
                        (lambda kt, q0, ncols, bS: lambda e: e.matmul(
                            ps[bS][:, :ncols], lhsT=kT[:, kt * 128:(kt + 1) * 128], rhs=qT[:, q0:q0 + ncols],
                            start=True, stop=False))(kt, q0, ncols, bS),
                        (lambda nb, q0, ncols, bS, diag: lambda e: e.matmul(
                            ps[bS][:, :ncols], lhsT=ct["enb"][:, nb, :], rhs=augb[:, q0:q0 + ncols],
                            start=False, stop=(not diag)))(nb, q0, ncols, bS, diag),
                    ]
                    if diag:
                        fns.append((lambda ncols, bS: lambda e: e.matmul(
                            ps[bS][:, :ncols], lhsT=ct["ident"][:], rhs=ct["maskB"][:, :ncols],
                            start=False, stop=True))(ncols, bS))
                    sc.op("pe", fns, reads=[KB[kt // 4], QB[Q], AG, CB], writes=[PB[bS]])

                def b_EPV(i, h=h):
                    Q, kt = steps[i]
                    half = kt % 2
                    q0, ncols, off, diag = geo(Q, kt)
                    bS = sbank[i]
                    bO = 2 * (Q % 2)
                    bD = bO + 1
                    nkt = 4 * Q + 4
                    sl = i % 3
                    ac((lambda sl, ncols, bS, h, half: lambda e: e.activation(
                        out=PTb[sl][:, :ncols], in_=ps[bS][:, :ncols], func=AF.Exp,
                        bias=prm[:, 32 + 2 * h + half:33 + 2 * h + half], scale=1.0))(sl, ncols, bS, h, half),
                       reads=[PB[bS], PRM], writes=[PTB_[sl]])
                    fns = [
                        (lambda kt, sl, ncols, off, bO, nkt: lambda e: e.matmul(
                            ps[bO][:, off:512], lhsT=vB[:, kt, :], rhs=PTb[sl][:, :ncols],
                            start=(kt == 0), stop=(kt == nkt - 1)))(kt, sl, ncols, off, bO, nkt),
                        (lambda kt, sl, ncols, off, bD, nkt: lambda e: e.matmul(
                            ps[bD][:, off:512], lhsT=ct["ones128"][:], rhs=PTb[sl][:, :ncols],
                            start=(kt == 0), stop=(kt == nkt - 1)))(kt, sl, ncols, off, bD, nkt),
                    ]
                    sc.op("pe", fns, reads=[PTB_[sl], VB[kt // 4], CB], writes=[PB[bO], PB[bD]])
                    if kt == nkt - 1:
                        dv((lambda bD: lambda e: e.reciprocal(out=rec[:], in_=ps[bD][:]))(bD),
                           reads=[PB[bD]], writes=[REC])
                        dv((lambda bO, h, Q: lambda e: e.tensor_tensor(
                            out=obT[:, h, Q * 512:(Q + 1) * 512], in0=ps[bO][:], in1=rec[:], op=ALU.mult))(bO, h, Q),
                           reads=[PB[bO], REC], writes=[OB[h][Q]])

                b_S(0)
                for i in range(len(steps)):
                    if i + 1 < len(steps):
                        b_S(i + 1)
                    b_EPV(i)
            sc.barrier()

        if DEBUG:
            sc.dma("sp", lambda e: e.dma_start(out=dbg["oaT"], in_=oaT[:].rearrange("p c t -> p (c t)")), chd,
                   reads=[b for r_ in OA for b in r_])
            sc.dma("sp", lambda e: e.dma_start(out=dbg["obT"], in_=obT[:].rearrange("p c t -> p (c t)")), chd,
                   reads=[b for r_ in OB for b in r_])

        OD = [[Buf("od%d_%d" % (t, c)) for c in range(16)] for t in range(NT)]

        with contextlib.ExitStack() as ph:
            mixT = sb(ph, "m_mix", [128, KC, 512], BF16)
            MX = [Buf("m_mx%d" % i) for i in range(KC)]
            wga = [sb(ph, "m_wga%d" % i, [128, 16, 128], BF16) for i in range(2)]
            wgb = [sb(ph, "m_wgb%d" % i, [128, 16, 128], BF16) for i in range(2)]
            wa = [sb(ph, "m_wa%d" % i, [128, 8, 128], BF16) for i in range(2)]
            wb = [sb(ph, "m_wb%d" % i, [128, 8, 128], BF16) for i in range(2)]
            WM = [Buf("m_w%d" % i) for i in range(2)]
            chm = [sc.chan("m_w%d" % i) for i in range(2)]
            wot = [sb(ph, "m_wo%d" % i, [128, 16, WOC], BF16) for i in range(2)]
            WO = [Buf("m_wo%d" % i) for i in range(2)]
            cho = [sc.chan("m_wo%d" % i) for i in range(2)]
            sga = sb(ph, "m_sga", [128, 512], F32)
            sgb = sb(ph, "m_sgb", [128, 512], F32)
            SG = [Buf("m_sga"), Buf("m_sgb")]
            NXR = 4
            xr = [sb(ph, "m_xr%d" % i, [128, WOC], F32) for i in range(NXR)]
            XR = [Buf("m_xr%d" % i) for i in range(NXR)]
            chx = [sc.chan("m_xr%d" % i) for i in range(NXR)]
            hs = [sb(ph, "m_hs%d" % i, [128, WOC], F32) for i in range(NXR)]
            HS = [Buf("m_hs%d" % i) for i in range(NXR)]
            chh = [sc.chan("m_hs%d" % i) for i in range(NXR)]
            store_chans = list(chh)

            jobs = []
            for tg in range(4):
                for oc in range(16):
                    jobs.append(("m", tg, oc))
                for cp in range(D // WOC):
                    jobs.append(("o", tg, cp))
            cntm = [0]
            cnto = [0]
            slot_of = {}
            for jb in jobs:
                if jb[0] == "m":
                    slot_of[jb] = cntm[0] % 2
                    cntm[0] += 1
                else:
                    slot_of[jb] = cnto[0] % 2
                    cnto[0] += 1

            def m_load(jb):
                s_ = slot_of[jb]
                if jb[0] == "m":
                    oc = jb[2]
                    for (t_, src) in ((wga, win[36 + oc]), (wgb, win[52 + oc]), (wa, wba[oc]), (wb, wbb[oc])):
                        sc.dma("pool", (lambda t_, src, s_: lambda e: e.dma_start(
                            out=t_[s_][:].rearrange("p k n -> p (k n)"), in_=src))(t_, src, s_),
                            chm[s_], writes=[WM[s_]])
                else:
                    cp = jb[2]
                    sc.dma("pool", (lambda cp, s_: lambda e: e.dma_start(
                        out=wot[s_][:].rearrange("p k n -> p (k n)"), in_=wo[cp]))(cp, s_), cho[s_], writes=[WO[s_]])

            bankset = [0]
            xc = [0]

            def m_compute(jb):
                s_ = slot_of[jb]
                tg = jb[1]
                tok = slice(tg * 512, (tg + 1) * 512)
                if jb[0] == "m":
                    oc = jb[2]
                    b0 = 4 * (bankset[0] % 2)
                    bankset[0] += 1
                    fns = []
                    for (bk, wt, src, nk) in ((b0, wga, uT, 16), (b0 + 1, wgb, uT, 16), (b0 + 2, wa, oaT, 8),
                                              (b0 + 3, wb, obT, 8)):
                        for kc in range(nk):
                            fns.append((lambda bk, wt, src, nk, kc: lambda e: e.matmul(
                                ps[bk][:], lhsT=wt[s_][:, kc, :], rhs=src[:, kc, tok],
                                start=(kc == 0), stop=(kc == nk - 1)))(bk, wt, src, nk, kc))
                    rds = [WM[s_]] + tg_bufs(tg) + [OA[c][tg] for c in range(8)] + [OB[c][tg] for c in range(8)]
                    sc.op("pe", fns, reads=rds, writes=[PB[b0], PB[b0 + 1], PB[b0 + 2], PB[b0 + 3]])
                    ac(lambda e: e.activation(out=sga[:], in_=ps[b0][:], func=AF.Sigmoid,
                                              bias=small[:, 12 + oc:13 + oc], scale=1.0),
                       reads=[PB[b0], CB], writes=[SG[0]])
                    ac(lambda e: e.activation(out=sgb[:], in_=ps[b0 + 1][:], func=AF.Sigmoid,
                                              bias=small[:, 28 + oc:29 + oc], scale=1.0),
                       reads=[PB[b0 + 1], CB], writes=[SG[1]])
                    dv(lambda e: e.tensor_tensor(out=sga[:], in0=sga[:], in1=ps[b0 + 2][:], op=ALU.mult),
                       reads=[SG[0], PB[b0 + 2]], writes=[SG[0]])
                    dv(lambda e: e.tensor_tensor(out=sgb[:], in0=sgb[:], in1=ps[b0 + 3][:], op=ALU.mult),
                       reads=[SG[1], PB[b0 + 3]], writes=[SG[1]])
                    dv(lambda e: e.tensor_tensor(out=mixT[:, oc, :], in0=sga[:], in1=sgb[:], op=ALU.add),
                       reads=SG, writes=[MX[oc]])
                else:
                    cp = jb[2]
                    cols = slice(cp * WOC, (cp + 1) * WOC)
                    for tt in range(4):
                        t = tg * 4 + tt
                        i2 = xc[0] % NXR
                        xc[0] += 1
                        rows = slice(t * 128, (t + 1) * 128)
                        sc.dma("act", (lambda i2, rows: lambda e: e.dma_start(out=xr[i2][:], in_=x[rows, cols]))(i2, rows),
                               chx[i2], writes=[XR[i2]])
                        bk = (xc[0]) % 8
                        fns = [(lambda kc, tt, bk: lambda e: e.matmul(
                            ps[bk][:, :WOC], lhsT=mixT[:, kc, tt * 128:(tt + 1) * 128], rhs=wot[s_][:, kc, :],
                            start=(kc == 0), stop=(kc == KC - 1)))(kc, tt, bk) for kc in range(KC)]
                        sc.op("pe", fns, reads=MX + [WO[s_]], writes=[PB[bk]])
                        dv((lambda i2, bk: lambda e: e.tensor_tensor(out=hs[i2][:], in0=ps[bk][:, :WOC], in1=xr[i2][:],
                                                                     op=ALU.add))(i2, bk),
                           reads=[PB[bk], XR[i2]], writes=[HS[i2]])
                        ods = [OD[t][cc] for cc in range(cp * WOC // 128, (cp + 1) * WOC // 128)]
                        sc.dma("sp", (lambda i2, rows: lambda e: e.dma_start(out=out[rows, cols], in_=hs[i2][:]))(i2, rows),
                               chh[i2], reads=[HS[i2]], writes=ods)
                        if DEBUG:
                            sc.dma("sp", (lambda i2, rows: lambda e: e.dma_start(out=dbg["h1"][rows, cols], in_=hs[i2][:]))(i2, rows),
                                   chh[i2], reads=[HS[i2]])

            m_load(jobs[0])
            for i, jb in enumerate(jobs):
                if i + 1 < len(jobs):
                    m_load(jobs[i + 1])
                m_compute(jb)
            sc.barrier()

        pers.close()

        with contextlib.ExitStack() as ph:
            T4 = 1024
            u2T = sb(ph, "f_u2T", [128, KC, T4], BF16)
            U2 = [Buf("f_u2%d" % i) for i in range(T4 // 128)]
            actT = sb(ph, "f_act", [128, FC, T4], BF16)
            ACT_ = [[Buf("f_act%d_%d" % (oc, i)) for i in range(2)] for oc in range(FC)]
            wgt = [sb(ph, "f_wg%d" % i, [128, 16, 128], BF16) for i in range(2)]
            wut = [sb(ph, "f_wu%d" % i, [128, 16, 128], BF16) for i in range(2)]
            WGU = [Buf("f_wgu%d" % i) for i in range(2)]
            chgu = [sc.chan("f_gu%d" % i) for i in range(2)]
            wdt = [sb(ph, "f_wd%d" % i, [128, FC, 128], BF16) for i in range(2)]
            WD = [Buf("f_wd%d" % i) for i in range(2)]
            chd_ = [sc.chan("f_wd%d" % i) for i in range(2)]
            sg = [sb(ph, "f_sg%d" % i, [128, 512], F32) for i in range(2)]
            SGf = [Buf("f_sg%d" % i) for i in range(2)]
            yT = [sb(ph, "f_yT%d" % i, [128, 512], F32) for i in range(2)]
            YT = [Buf("f_yT%d" % i) for i in range(2)]
            h1r = [sb(ph, "f_h1r%d" % i, [128, 4, 128], F32) for i in range(2)]
            H1R = [Buf("f_h1r%d" % i) for i in range(2)]
            chr_ = [sc.chan("f_h1r%d" % i) for i in range(2)]
            yo = [sb(ph, "f_yo%d" % i, [128, 4, 128], F32) for i in range(2)]
            YO = [Buf("f_yo%d" % i) for i in range(2)]
            chy = [sc.chan("f_yo%d" % i) for i in range(2)]
            store_chans2 = list(chy)
            outv = out.rearrange("(t p) f -> p t f", p=128)

            with contextlib.ExitStack() as ph_n:
                pass
            for hf in range(2):
                t0 = hf * (T4 // 128)
                with contextlib.ExitStack() as phn:
                    pass
                if hf == 0:
                    nstack = ph
                    n_xt = [sb(ph, "f_xt%d" % i, [128, D], F32) for i in range(2)]
                    n_xn1 = sb(ph, "f_xn0", [128, D], BF16)
                    n_xn = [n_xn1, n_xn1]
                    n_gb = sb(ph, "f_gb", [128, D], F32)
                    n_st = sb(ph, "f_st", [128, 3 * NT], F32)
                    NXT = [Buf("f_xt%d" % i) for i in range(2)]
                    NXN1 = Buf("f_xn0")
                    NXN = [NXN1, NXN1]
                    NGB = Buf("f_gb")
                    NST = [Buf("f_st%d" % i) for i in range(NT)]
                    chnx = [sc.chan("f_x%d" % i) for i in range(2)]
                    chng = sc.chan("f_g")
                    sc.dma("sp", lambda e: e.dma_start(out=n_gb[:], in_=g2b_d), chng, writes=[NGB])
                for tl in range(T4 // 128):
                    t = t0 + tl
                    s_ = t % 2
                    sc.dma("sp", (lambda s_, t: lambda e: e.dma_start(out=n_xt[s_][:], in_=out[t * 128:(t + 1) * 128, :]))(s_, t),
                           chnx[s_], reads=OD[t], writes=[NXT[s_]])
                    ac((lambda s_, t: lambda e: e.activation(out=n_xn[s_][:], in_=n_xt[s_][:], func=AF.Square,
                                                             accum_out=n_st[:, 3 * t:3 * t + 1]))(s_, t),
                       reads=[NXT[s_]], writes=[NXN[s_], NST[t]])
                    ac((lambda t: lambda e: e.activation(out=n_st[:, 3 * t + 1:3 * t + 2], in_=n_st[:, 3 * t:3 * t + 1],
                                                         func=AF.Ln, bias=ct_eps[:, 0:1], scale=1.0 / D))(t),
                       reads=[NST[t], PRM], writes=[NST[t]])
                    ac((lambda t: lambda e: e.activation(out=n_st[:, 3 * t + 2:3 * t + 3],
                                                         in_=n_st[:, 3 * t + 1:3 * t + 2], func=AF.Exp, scale=-0.5))(t),
                       reads=[NST[t]], writes=[NST[t]])
                    dv((lambda s_, t: lambda e: e.scalar_tensor_tensor(
                        out=n_xn[s_][:], in0=n_xt[s_][:], scalar=n_st[:, 3 * t + 2:3 * t + 3], in1=n_gb[:],
                        op0=ALU.mult, op1=ALU.mult))(s_, t), reads=[NXT[s_], NST[t], NGB], writes=[NXN[s_]])
                    for half in range(2):
                        bk = (2 * t + half) % 8
                        pbf = ps[bk][:].bitcast(BF16)
                        fns = []
                        for jj in range(8):
                            c = half * 8 + jj
                            fns.append((lambda s_, c, jj, pbf: lambda e: e.transpose(
                                out=pbf[:, jj * 128:(jj + 1) * 128], in_=n_xn[s_][:, c * 128:(c + 1) * 128],
                                identity=ct["ident"][:]))(s_, c, jj, pbf))
                        sc.op("pe", fns, reads=[NXN[s_], CB], writes=[PB[bk]])
                        dstv = u2T[:, half * 8:(half + 1) * 8, tl * 128:(tl + 1) * 128]
                        srcv = pbf[:, 0:1024].rearrange("p (c t) -> p c t", c=8)
                        if half == 0:
                            ac((lambda dstv, srcv: lambda e: e.activation(out=dstv, in_=srcv, func=AF.Copy))(dstv, srcv),
                               reads=[PB[bk]], writes=[U2[tl]])
                        else:
                            dv((lambda dstv, srcv: lambda e: e.tensor_copy(out=dstv, in_=srcv))(dstv, srcv),
                               reads=[PB[bk]], writes=[U2[tl]])

                def gu_load(oc):
                    s_ = oc % 2
                    for (t_, src) in ((wgt, wg[oc]), (wut, wu[oc])):
                        sc.dma("pool", (lambda t_, src, s_: lambda e: e.dma_start(
                            out=t_[s_][:].rearrange("p k n -> p (k n)"), in_=src))(t_, src, s_),
                            chgu[s_], writes=[WGU[s_]])

                def wd_load(oc):
                    s_ = oc % 2
                    sc.dma("pool", (lambda oc, s_: lambda e: e.dma_start(
                        out=wdt[s_][:].rearrange("p k n -> p (k n)"), in_=wd[oc]))(oc, s_), chd_[s_], writes=[WD[s_]])

                gu_load(0)
                bc = [0]
                for oc in range(FC):
                    s_ = oc % 2
                    if oc + 1 < FC:
                        gu_load(oc + 1)
                    elif True:
                        wd_load(0)
                    for t2 in range(2):
                        bG_ = 2 * (bc[0] % 4)
                        bU_ = bG_ + 1
                        i2 = bc[0] % 2
                        bc[0] += 1
                        tok = slice(t2 * 512, (t2 + 1) * 512)
                        fns = []
                        for (bk, wt) in ((bG_, wgt), (bU_, wut)):
                            for kc in range(KC):
                                fns.append((lambda bk, wt, kc, s_, tok: lambda e: e.matmul(
                                    ps[bk][:], lhsT=wt[s_][:, kc, :], rhs=u2T[:, kc, tok],
                                    start=(kc == 0), stop=(kc == KC - 1)))(bk, wt, kc, s_, tok))
                        sc.op("pe", fns, reads=[WGU[s_]] + U2[t2 * 4:(t2 + 1) * 4], writes=[PB[bG_], PB[bU_]])
                        ac((lambda i2, bG_: lambda e: e.activation(out=sg[i2][:], in_=ps[bG_][:], func=AF.Silu))(i2, bG_),
                           reads=[PB[bG_]], writes=[SGf[i2]])
                        dv((lambda i2, bU_, oc, tok: lambda e: e.tensor_tensor(
                            out=actT[:, oc, tok], in0=sg[i2][:], in1=ps[bU_][:], op=ALU.mult))(i2, bU_, oc, tok),
                           reads=[SGf[i2], PB[bU_]], writes=[ACT_[oc][t2]])
                steps_c = [(oc, t2) for oc in range(16) for t2 in range(2)]
                cinfo = {}

                def c_mm(j, t0=t0):
                    oc, t2 = steps_c[j]
                    s_ = oc % 2
                    if t2 == 0 and oc + 1 < 16:
                        wd_load(oc + 1)
                    i2 = bc[0] % 2
                    bY = 2 * (bc[0] % 4)
                    bT = bY + 1
                    bc[0] += 1
                    tok = slice(t2 * 512, (t2 + 1) * 512)
                    tts = slice(t0 + t2 * 4, t0 + t2 * 4 + 4)
                    ods = [OD[t][oc] for t in range(t0 + t2 * 4, t0 + t2 * 4 + 4)]
                    cinfo[j] = (i2, bY, bT, tts, ods, oc)
                    sc.dma("act", (lambda i2, tts, oc: lambda e: e.dma_start(
                        out=h1r[i2][:], in_=outv[:, tts, oc * 128:(oc + 1) * 128]))(i2, tts, oc),
                        chr_[i2], reads=ods, writes=[H1R[i2]])
                    fns = [(lambda kc, s_, tok, bY, oc: lambda e: e.matmul(
                        ps[bY][:], lhsT=wdt[s_][:, kc, :], rhs=actT[:, kc, tok],
                        start=(kc == 0), stop=(kc == FC - 1)))(kc, s_, tok, bY, oc) for kc in range(FC)]
                    sc.op("pe", fns, reads=[WD[s_]] + [ACT_[k][t2] for k in range(FC)], writes=[PB[bY]])

                def c_tail(j):
                    i2, bY, bT, tts, ods, oc = cinfo[j]
                    ac((lambda i2, bY: lambda e: e.activation(out=yT[i2][:], in_=ps[bY][:], func=AF.Copy))(i2, bY),
                       reads=[PB[bY]], writes=[YT[i2]])
                    fns = [(lambda jj, i2, bT: lambda e: e.transpose(
                        out=ps[bT][:, jj * 128:(jj + 1) * 128], in_=yT[i2][:, jj * 128:(jj + 1) * 128],
                        identity=ct["identf"][:]))(jj, i2, bT) for jj in range(4)]
                    sc.op("pe", fns, reads=[YT[i2], CB], writes=[PB[bT]])
                    dv((lambda i2, bT: lambda e: e.tensor_tensor(
                        out=yo[i2][:], in0=ps[bT][:].rearrange("p (t f) -> p t f", t=4), in1=h1r[i2][:],
                        op=ALU.add))(i2, bT), reads=[PB[bT], H1R[i2]], writes=[YO[i2]])
                    sc.dma("sp", (lambda i2, tts, oc: lambda e: e.dma_start(
                        out=outv[:, tts, oc * 128:(oc + 1) * 128], in_=yo[i2][:]))(i2, tts, oc),
                        chy[i2], reads=[YO[i2]], writes=ods)

                c_mm(0)
                for j in range(len(steps_c)):
                    if j + 1 < len(steps_c):
                        c_mm(j + 1)
                    c_tail(j)
            sc.barrier()

        with nc.Block() as block:
            @block.tensor
            def _(eng):
                sc.replay("pe", eng)

            @block.scalar
            def _(eng):
                sc.replay("act", eng)

            @block.vector
            def _(eng):
                sc.replay("dve", eng)

            @block.gpsimd
            def _(eng):
                sc.replay("pool", eng)

            @block.sync
            def _(eng):
                sc.replay("sp", eng)
    return nc


def _tile_w(w, kc, ncols, width):
    return np.ascontiguousarray(
        w.reshape(kc, 128, ncols, width).transpose(2, 1, 0, 3)).reshape(ncols, 128, kc * width)


_CACHE = {}


def kernel(x, norm1_g, w_in, b_gate, q_norm_a, k_norm_a, sinks_a, q_norm_b, k_norm_b,
           w_branch_a, w_branch_b, w_o, norm2_g, w_ffn_gate, w_ffn_up, w_ffn_down):
    f32 = np.float32
    x = np.asarray(x, f32)
    consts = make_consts()
    if "nc" not in _CACHE:
        _CACHE["nc"] = build_nc(consts)
    nc = _CACHE["nc"]

    shared = {}
    shared["win_t"] = _tile_w(np.asarray(w_in, f32)[0], 16, 68, 128)
    shared["wba_t"] = _tile_w(np.asarray(w_branch_a, f32)[0], 8, 16, 128)
    shared["wbb_t"] = _tile_w(np.asarray(w_branch_b, f32)[0], 8, 16, 128)
    shared["wo_t"] = _tile_w(np.asarray(w_o, f32)[0], 16, D // WOC, WOC)
    shared["wg_t"] = _tile_w(np.asarray(w_ffn_gate, f32)[0], 16, FC, 128)
    shared["wu_t"] = _tile_w(np.asarray(w_ffn_up, f32)[0], 16, FC, 128)
    shared["wd_t"] = _tile_w(np.asarray(w_ffn_down, f32)[0], FC, 16, 128)
    shared["g1b"] = np.ascontiguousarray(np.broadcast_to(np.asarray(norm1_g, f32)[0][None, :], (128, D)))
    shared["g2b"] = np.ascontiguousarray(np.broadcast_to(np.asarray(norm2_g, f32)[0][None, :], (128, D)))
    small = np.zeros((128, 64), f32)
    qa = np.asarray(q_norm_a, f32)[0]
    ka = np.asarray(k_norm_a, f32)[0]
    qb = np.asarray(q_norm_b, f32)[0]
    kb = np.asarray(k_norm_b, f32)[0]
    small[:64, 0] = qa
    small[64:, 0] = qa
    small[:64, 1] = ka
    small[64:, 1] = ka
    small[:, 2] = qb
    small[:, 3] = kb
    sk = np.asarray(sinks_a, f32)[0]
    for c in range(8):
        small[:64, 4 + c] = sk[2 * c]
        small[64:, 4 + c] = sk[2 * c + 1]
    small[:, 12:44] = np.asarray(b_gate, f32)[0].reshape(32, 128).T
    shared["small"] = small
    gbro = np.zeros((128, 384), f32)
    gbro[:, 0:64] = qa[None, :]
    gbro[:, 64:128] = ka[None, :]
    gbro[:, 128:256] = qb[None, :]
    gbro[:, 256:384] = kb[None, :]
    shared["gbro"] = gbro
    for k, v in consts.items():
        shared["c_" + k] = v

    in_maps = []
    for b in range(8):
        m = dict(shared)
        m["x"] = np.ascontiguousarray(x[b])
        in_maps.append(m)
    res = run_bass_kernel_spmd(nc, in_maps, core_ids=list(range(8)))
    _CACHE["last"] = res
    outp = np.stack([np.asarray(r["out"], f32) for r in res.results], axis=0)
    return outp
```

```python
import contextlib
import numpy as np
import ml_dtypes
import concourse.bass as bass
import concourse.mybir as mybir
from concourse.bass_utils import run_bass_kernel_spmd

F32 = mybir.dt.float32
BF16 = mybir.dt.bfloat16
AF = mybir.ActivationFunctionType
ALU = mybir.AluOpType
AX = mybir.AxisListType

S = 2048
D = 2048
NT = S // 128
KC = D // 128
DFF = 5632
FC = DFF // 128
EPS = 1e-6
NEG = -30000.0
WOC = 256
DEBUG = False


def slopes(n):
    return np.exp2(-8.0 * np.arange(1, n + 1, dtype=np.float64) / n)


class Buf:
    __slots__ = ("name", "w", "r")

    def __init__(self, name):
        self.name = name
        self.w = None
        self.r = {}


class Chan:
    def __init__(self, sem):
        self.sem = sem
        self.count = 0


ENGS = ["pe", "act", "dve", "pool", "sp"]


class Sched:
    def __init__(self, nc, stack):
        self.nc = nc
        self.stack = stack
        self.plan = {e: [] for e in ENGS}
        self.cnt = {e: 0 for e in ENGS}
        self.seen = {e: {} for e in ENGS}
        self.sem = {e: stack.enter_context(nc.semaphore("sem_" + e)) for e in ENGS}
        self.semobj = {}
        for e in ENGS:
            self.semobj["E" + e] = self.sem[e]
        self.chans = []

    def chan(self, name):
        sem = self.stack.enter_context(self.nc.semaphore("ch_" + name))
        c = Chan(sem)
        c.key = "C%d" % len(self.chans)
        self.semobj[c.key] = sem
        self.chans.append(c)
        return c

    def _deps(self, reads, writes):
        deps = {}

        def add(k, v):
            if deps.get(k, 0) < v:
                deps[k] = v
        for b in reads:
            if b.w is not None:
                add(*b.w)
        for b in writes:
            if b.w is not None:
                add(*b.w)
            for k, v in b.r.items():
                add(k, v)
        return deps

    def _waits(self, e, deps):
        for k, v in deps.items():
            if self.seen[e].get(k, 0) < v:
                self.plan[e].append(("wait", self.semobj[k], v))
                self.seen[e][k] = v

    def _commit(self, key, val, reads, writes):
        for b in reads:
            if b.r.get(key, 0) < val:
                b.r[key] = val
        for b in writes:
            b.w = (key, val)
            b.r = {}

    def op(self, e, fns, reads=(), writes=()):
        if not isinstance(fns, (list, tuple)):
            fns = [fns]
        self._waits(e, self._deps(reads, writes))
        self.cnt[e] += 1
        self.plan[e].append(("ops", list(fns), self.sem[e]))
        self._commit("E" + e, self.cnt[e], reads, writes)

    def dma(self, q, fn, ch, reads=(), writes=()):
        deps = self._deps(reads, writes)
        own = [b for b in writes if b.w is not None and b.w[0] == ch.key and not b.r]
        if own and all(b.w is None or b.w[0] == ch.key for b in writes):
            only_own = True
            for b in reads:
                if b.w is not None and b.w[0] == ch.key:
                    only_own = False
            if only_own:
                deps.pop(ch.key, None)
        self._waits(q, deps)
        ch.count += 16
        self.plan[q].append(("dma", fn, ch.sem))
        self._commit(ch.key, ch.count, reads, writes)

    def barrier(self):
        for e in ENGS:
            deps = {}
            for o in ENGS:
                if self.cnt[o] > 0:
                    deps["E" + o] = self.cnt[o]
            for c in self.chans:
                if c.count > 0:
                    deps[c.key] = c.count
            self._waits(e, deps)

    def replay(self, e, eng):
        for it in self.plan[e]:
            if it[0] == "wait":
                eng.wait_ge(it[1], it[2])
            elif it[0] == "ops":
                fns = it[1]
                for f in fns[:-1]:
                    f(eng)
                fns[-1](eng).then_inc(it[2], 1)
            else:
                it[1](eng).then_inc(it[2], 16)


def split_bf16(x, n):
    parts = []
    r = np.asarray(x, np.float64)
    for _ in range(n):
        p = r.astype(np.float32).astype(ml_dtypes.bfloat16)
        parts.append(p)
        r = r - p.astype(np.float64)
    return parts


def make_consts():
    bf = ml_dtypes.bfloat16
    c = {}
    c["ident"] = np.eye(128, dtype=np.float32).astype(bf)
    c["identf"] = np.eye(128, dtype=np.float32)
    j = np.arange(128)[:, None]
    cc = np.arange(256)[None, :]
    dist = cc - j
    c["maskA"] = np.where((dist >= 0) & (dist < 128), 0.0, NEG).astype(np.float32).astype(bf)
    mb = np.zeros((128, 512), np.float32)
    i = np.arange(128)[None, :]
    mb[:, :128] = np.where(j <= i, 0.0, NEG)
    c["maskB"] = mb.astype(bf)
    o3 = np.zeros((128, 128), np.float32)
    o3[0:3, :] = 1.0
    c["ones3"] = o3.astype(bf)
    sa = slopes(16)
    at = np.zeros((128, 16, 256), dtype=bf)
    for h in range(16):
        parts = split_bf16(-sa[h] * np.arange(256, dtype=np.float64), 3)
        for k in range(3):
            at[k, h, :] = parts[k]
    c["atab"] = at
    en = np.zeros((128, 8, 128), np.float32)
    for nb in range(8):
        en[nb, nb, :] = 1.0
        en[8 + nb, nb, :] = 1.0
    c["enb"] = en.astype(bf)
    bo = np.zeros((128, 128), np.float32)
    bo[:64, :64] = 1.0
    bo[64:, 64:] = 1.0
    c["blkones"] = bo.astype(bf)
    c["ones128"] = np.ones((128, 128), np.float32).astype(bf)
    lo = np.zeros((128, 128), np.float32)
    lo[:, :64] = 1.0
    hi = np.zeros((128, 128), np.float32)
    hi[:, 64:] = 1.0
    c["oneslo"] = lo.astype(bf)
    c["oneshi"] = hi.astype(bf)
    p = np.arange(128, dtype=np.float64)
    c["slpA"] = (p[:, None] * sa[None, :]).astype(np.float32)
    sb = slopes(8)
    slb = np.zeros((128, 8, 2), np.float64)
    for h in range(8):
        for half in range(2):
            slb[:, h, half] = sb[h] * (half * 128 + p)
    c["slpB"] = slb.astype(np.float32).reshape(128, 16)
    tq = np.zeros((128, 16, 8), np.float64)
    cm = np.zeros((128, 16, 8), np.float32)
    no = np.ones((128, 16, 8), np.float32)
    for n in range(16):
        for nb in range(8):
            tq[:, n, nb] = n * 128 + p - 256 * nb
            if nb >= n // 2:
                cm[:, n, nb] = -1e30
        no[:, n, n // 2] = 0.0
    c["tqrel"] = tq.astype(np.float32).reshape(128, 128)
    c["cmask"] = cm.reshape(128, 128)
    c["notown"] = no.reshape(128, 128)
    return c


CONST_SHAPES = None


def build_nc(consts):
    nc = bass.Bass("TRN2", target_bir_lowering=False)
    sA = slopes(16)
    sB = slopes(8)

    def din(name, shape, dt=F32):
        return nc.dram_tensor(name, list(shape), dt, kind="ExternalInput").ap()

    x = din("x", [S, D])
    win = din("win_t", [68, 128, 16 * 128])
    wba = din("wba_t", [16, 128, 8 * 128])
    wbb = din("wbb_t", [16, 128, 8 * 128])
    wo = din("wo_t", [D // WOC, 128, 16 * WOC])
    wg = din("wg_t", [FC, 128, 16 * 128])
    wu = din("wu_t", [FC, 128, 16 * 128])
    wd = din("wd_t", [16, 128, FC * 128])
    g1b_d = din("g1b", [128, D])
    g2b_d = din("g2b", [128, D])
    small_d = din("small", [128, 64])
    cd = {}
    for k, v in consts.items():
        cd[k] = din("c_" + k, v.shape, BF16 if v.dtype == ml_dtypes.bfloat16 else F32)
    out = nc.dram_tensor("out", [S, D], F32, kind="ExternalOutput").ap()
    dbg = {}
    if DEBUG:
        dbg["uT"] = nc.dram_tensor("dbg_uT", [128, KC * S], BF16, kind="ExternalOutput").ap()
        dbg["oaT"] = nc.dram_tensor("dbg_oaT", [128, 8 * S], BF16, kind="ExternalOutput").ap()
        dbg["obT"] = nc.dram_tensor("dbg_obT", [128, 8 * S], BF16, kind="ExternalOutput").ap()
        dbg["h1"] = nc.dram_tensor("dbg_h1", [S, D], F32, kind="ExternalOutput").ap()
        for nm in ("klo", "khi", "qTa", "vlo", "vhi"):
            dbg[nm] = nc.dram_tensor("dbg_" + nm, [128, S], BF16, kind="ExternalOutput").ap()
        for nm in ("pt0", "pt1"):
            dbg[nm] = nc.dram_tensor("dbg_" + nm, [128, 256], BF16, kind="ExternalOutput").ap()
        dbg["prm"] = nc.dram_tensor("dbg_prm", [128, 64], F32, kind="ExternalOutput").ap()

    with contextlib.ExitStack() as top:
        sc = Sched(nc, top)

        def sb(stack, name, shape, dt):
            t = stack.enter_context(nc.sbuf_tensor("sb_" + name, list(shape), dt))
            return t

        ps = [top.enter_context(nc.psum_tensor("ps%d" % i, [128, 512], F32)) for i in range(8)]
        PB = [Buf("ps%d" % i) for i in range(8)]

        UT = [Buf("uT%d" % t) for t in range(NT)]
        OA = [[Buf("oa%d_%d" % (c, q)) for q in range(4)] for c in range(8)]
        OB = [[Buf("ob%d_%d" % (h, q)) for q in range(4)] for h in range(8)]

        ct = {}
        PH_CONSTS = {"atab": 1, "maskA": 1, "enb": 2, "maskB": 2, "tqrel": 2, "cmask": 2, "notown": 2}

        def alloc_consts(stack, names):
            for k in names:
                v = consts[k]
                ct[k] = sb(stack, "k_" + k, v.shape, BF16 if v.dtype == ml_dtypes.bfloat16 else F32)

        def load_consts(names):
            for k in names:
                sc.dma("sp", (lambda k: lambda e: e.dma_start(out=ct[k][:], in_=cd[k]))(k), chc, writes=[CB])

        GLOB = [k for k in consts if k not in PH_CONSTS]
        alloc_consts(top, GLOB)
        small = sb(top, "small", [128, 64], F32)
        prm = sb(top, "prm", [128, 64], F32)
        CB = Buf("consts")
        PRM = Buf("prm")
        chc = sc.chan("const")

        gbro = sb(top, "gbro", [128, 384], F32)
        gbro_d = din("gbro", [128, 384])

        load_consts(GLOB)
        sc.dma("sp", lambda e: e.dma_start(out=small[:], in_=small_d), chc, writes=[CB])
        sc.dma("sp", lambda e: e.dma_start(out=gbro[:], in_=gbro_d), chc, writes=[CB])

        def dv(f, reads=(), writes=()):
            sc.op("dve", f, reads, writes)

        def ac(f, reads=(), writes=()):
            sc.op("act", f, reads, writes)

        for i, (lo_, hi_) in enumerate([(0, 64), (64, 128), (128, 256), (256, 384)]):
            dv((lambda i, lo_, hi_: lambda e: e.tensor_reduce(
                out=prm[:, 6 + i:7 + i], in_=gbro[:, lo_:hi_], axis=AX.X, op=ALU.max,
                apply_absolute_value=True))(i, lo_, hi_), reads=[CB], writes=[PRM])
        dv(lambda e: e.tensor_scalar(out=prm[:, 0:1], in0=small[:, 1:2], scalar1=8.0, scalar2=None, op0=ALU.mult),
           reads=[CB], writes=[PRM])
        dv(lambda e: e.tensor_scalar(out=prm[:, 1:2], in0=small[:, 3:4], scalar1=float(np.sqrt(128.0)),
                                     scalar2=None, op0=ALU.mult), reads=[CB], writes=[PRM])
        dv(lambda e: e.scalar_tensor_tensor(out=prm[:, 2:3], in0=prm[:, 6:7], scalar=8.0, in1=prm[:, 7:8],
                                            op0=ALU.mult, op1=ALU.mult), reads=[PRM], writes=[PRM])
        dv(lambda e: e.scalar_tensor_tensor(out=prm[:, 3:4], in0=prm[:, 8:9], scalar=float(np.sqrt(128.0)),
                                            in1=prm[:, 9:10], op0=ALU.mult, op1=ALU.mult), reads=[PRM], writes=[PRM])
        dv(lambda e: e.tensor_scalar(out=prm[:, 4:6], in0=prm[:, 2:4], scalar1=-1.0, scalar2=None, op0=ALU.mult),
           reads=[PRM], writes=[PRM])
        dv(lambda e: e.tensor_scalar(out=prm[:, 16:32], in0=ct["slpA"][:], scalar1=prm[:, 2:3], scalar2=None,
                                     op0=ALU.subtract), reads=[PRM, CB], writes=[PRM])
        dv(lambda e: e.tensor_scalar(out=prm[:, 32:48], in0=ct["slpB"][:], scalar1=prm[:, 3:4], scalar2=None,
                                     op0=ALU.subtract), reads=[PRM, CB], writes=[PRM])
        ac(lambda e: e.activation(out=prm[:, 48:56], in_=small[:, 4:12], func=AF.Exp, bias=prm[:, 4:5], scale=1.0),
           reads=[PRM, CB], writes=[PRM])

        def rmsnorm_to_T(stack, src_loader, gb_dram, dstT, DST, ntiles, tok0, tagp):
            xt = [sb(stack, tagp + "xt%d" % i, [128, D], F32) for i in range(3)]
            xn = [sb(stack, tagp + "xn%d" % i, [128, D], BF16) for i in range(3)]
            gbt = sb(stack, tagp + "gb", [128, D], F32)
            st = sb(stack, tagp + "st", [128, 3 * ntiles], F32)
            XT = [Buf(tagp + "xt%d" % i) for i in range(3)]
            XN = [Buf(tagp + "xn%d" % i) for i in range(3)]
            GB = Buf(tagp + "gb")
            ST = [Buf(tagp + "st%d" % i) for i in range(ntiles)]
            chx = [sc.chan(tagp + "x%d" % i) for i in range(3)]
            chg = sc.chan(tagp + "g")
            sc.dma("sp", lambda e: e.dma_start(out=gbt[:], in_=gb_dram), chg, writes=[GB])
            def front(t):
                s_ = t % 3
                src, srcbufs = src_loader(t)
                sc.dma("sp", (lambda s_, src: lambda e: e.dma_start(out=xt[s_][:], in_=src))(s_, src),
                       chx[s_], reads=srcbufs, writes=[XT[s_]])
                ac((lambda s_, t: lambda e: e.activation(out=xn[s_][:], in_=xt[s_][:], func=AF.Square,
                                                         accum_out=st[:, 3 * t:3 * t + 1]))(s_, t),
                   reads=[XT[s_]], writes=[XN[s_], ST[t]])
                ac((lambda t: lambda e: e.activation(out=st[:, 3 * t + 1:3 * t + 2], in_=st[:, 3 * t:3 * t + 1],
                                                     func=AF.Ln, bias=ct_eps[:, 0:1], scale=1.0 / D))(t),
                   reads=[ST[t], PRM], writes=[ST[t]])
                ac((lambda t: lambda e: e.activation(out=st[:, 3 * t + 2:3 * t + 3], in_=st[:, 3 * t + 1:3 * t + 2],
                                                     func=AF.Exp, scale=-0.5))(t),
                   reads=[ST[t]], writes=[ST[t]])
                dv((lambda s_, t: lambda e: e.scalar_tensor_tensor(
                    out=xn[s_][:], in0=xt[s_][:], scalar=st[:, 3 * t + 2:3 * t + 3], in1=gbt[:],
                    op0=ALU.mult, op1=ALU.mult))(s_, t),
                   reads=[XT[s_], ST[t], GB], writes=[XN[s_]])

            def back(t):
                s_ = t % 3
                for half in range(2):
                    bk = 4 + (2 * t + half) % 4
                    pbf = ps[bk][:].bitcast(BF16)
                    fns = []
                    for jj in range(8):
                        c = half * 8 + jj
                        fns.append((lambda s_, c, jj, pbf: lambda e: e.transpose(
                            out=pbf[:, jj * 128:(jj + 1) * 128], in_=xn[s_][:, c * 128:(c + 1) * 128],
                            identity=ct["ident"][:]))(s_, c, jj, pbf))
                    sc.op("pe", fns, reads=[XN[s_], CB], writes=[PB[bk]])
                    dstv = dstT[:, half * 8:(half + 1) * 8, tok0 + t * 128: tok0 + (t + 1) * 128]
                    srcv = pbf[:, 0:1024].rearrange("p (c t) -> p c t", c=8)
                    if half == 0:
                        ac((lambda dstv, srcv: lambda e: e.activation(out=dstv, in_=srcv, func=AF.Copy))(dstv, srcv),
                           reads=[PB[bk]], writes=[DST[t]])
                    else:
                        dv((lambda dstv, srcv: lambda e: e.tensor_copy(out=dstv, in_=srcv))(dstv, srcv),
                           reads=[PB[bk]], writes=[DST[t]])

            front(0)
            for t in range(ntiles):
                if t + 1 < ntiles:
                    front(t + 1)
                back(t)

        ct_eps = sb(top, "ct_eps", [128, 4], F32)
        sc.op("pool", lambda e: e.memset(ct_eps[:, 0:1], EPS), writes=[PRM])
        sc.op("pool", lambda e: e.memset(ct_eps[:, 1:2], EPS * 64), writes=[PRM])
        sc.op("pool", lambda e: e.memset(ct_eps[:, 2:3], EPS * 128), writes=[PRM])

        pers = contextlib.ExitStack()
        uT = sb(pers, "uT", [128, KC, S], BF16)
        oaT = sb(pers, "oaT", [128, 8, S], BF16)
        obT = sb(pers, "obT", [128, 8, S], BF16)

        with contextlib.ExitStack() as ph:
            rmsnorm_to_T(ph, lambda t: (x[t * 128:(t + 1) * 128, :], []), g1b_d, uT, UT, NT, 0, "p0")
            sc.barrier()

        if DEBUG:
            chd = sc.chan("dbg")
            sc.dma("sp", lambda e: e.dma_start(out=dbg["uT"], in_=uT[:].rearrange("p c t -> p (c t)")), chd,
                   reads=UT)

        def tg_bufs(tg):
            return UT[tg * 4:(tg + 1) * 4]

        def qknorm(psA_i, psB_i, ones_t, epscol, sq, SQ, lnv, LNV, gcol_ap, outs):
            ac(lambda e: e.activation(out=sq[:], in_=ps[psA_i][:], func=AF.Square),
               reads=[PB[psA_i]], writes=[SQ])
            sc.op("pe", lambda e: e.matmul(ps[psB_i][:], lhsT=ones_t[:], rhs=sq[:], start=True, stop=True),
                  reads=[SQ, CB], writes=[PB[psB_i]])
            ac(lambda e: e.activation(out=lnv[:], in_=ps[psB_i][:], func=AF.Ln, bias=ct_eps[:, epscol:epscol + 1],
                                      scale=1.0), reads=[PB[psB_i], PRM], writes=[LNV])
            ac(lambda e: e.activation(out=lnv[:], in_=lnv[:], func=AF.Exp, scale=-0.5), reads=[LNV], writes=[LNV])
            for (r0, r1, dst, dbufs, c0, c1, acc) in outs:
                def f(e, r0=r0, r1=r1, dst=dst, c0=c0, c1=c1, acc=acc):
                    kw = {}
                    if acc is not None:
                        kw["accum_out"] = acc
                    return e.scalar_tensor_tensor(out=dst, in0=ps[psA_i][r0:r1, c0:c1], scalar=gcol_ap[r0:r1, :],
                                                  in1=lnv[r0:r1, c0:c1], op0=ALU.mult, op1=ALU.mult, **kw)
                dv(f, reads=[PB[psA_i], LNV, PRM, CB], writes=dbufs)

        def load_w(stack_tile, src_ap, ch, WB, q="pool"):
            sc.dma(q, lambda e: e.dma_start(out=stack_tile, in_=src_ap), ch, writes=[WB])

        with contextlib.ExitStack() as ph:
            alloc_consts(ph, [k for k in PH_CONSTS if PH_CONSTS[k] == 1])
            load_consts([k for k in PH_CONSTS if PH_CONSTS[k] == 1])
            wk2 = [sb(ph, "a_wk%d" % i, [128, 16, 128], BF16) for i in range(2)]
            wv = [sb(ph, "a_wv%d" % i, [128, 16, 64], BF16) for i in range(2)]
            wq = [sb(ph, "a_wq%d" % i, [128, 16, 128], BF16) for i in range(2)]
            WKV = [Buf("a_wkv%d" % i) for i in range(2)]
            WQ = [Buf("a_wq%d" % i) for i in range(2)]
            chkv = [sc.chan("a_kv%d" % i) for i in range(2)]
            chq = [sc.chan("a_q%d" % i) for i in range(2)]
            Klo = sb(ph, "a_klo", [128, S], BF16)
            Khi = sb(ph, "a_khi", [128, S], BF16)
            vlo = sb(ph, "a_vlo", [128, NT, 128], BF16)
            vhi = sb(ph, "a_vhi", [128, NT, 128], BF16)
            qTa_ = sb(ph, "a_qT", [128, S], BF16)
            KB = [Buf("a_K%d" % i) for i in range(4)]
            VB = [Buf("a_V%d" % i) for i in range(2)]
            QB = [Buf("a_Q%d" % i) for i in range(4)]
            sq = [sb(ph, "a_sq%d" % i, [128, 512], BF16) for i in range(2)]
            lnv = [sb(ph, "a_ln%d" % i, [128, 512], F32) for i in range(2)]
            SQ = [Buf("a_sq%d" % i) for i in range(2)]
            LNV = [Buf("a_ln%d" % i) for i in range(2)]
            PT = [[sb(ph, "a_pt%d_%d" % (e_, i), [128, 256], BF16) for i in range(3)] for e_ in range(2)]
            PTB_ = [[Buf("a_pt%d_%d" % (e_, i)) for i in range(3)] for e_ in range(2)]
            den = sb(ph, "a_den", [128, 512], F32)
            DEN = Buf("a_den")

            sc.op("pool", lambda e: e.memset(Klo[64:128, :], 0.0), writes=KB)
            sc.op("pool", lambda e: e.memset(Khi[0:64, :], 0.0), writes=KB)
            sc.op("pool", lambda e: e.memset(vlo[:].rearrange("p t d -> p (t d)"), 0.0), writes=VB)
            sc.op("pool", lambda e: e.memset(vhi[:].rearrange("p t d -> p (t d)"), 0.0), writes=VB)

            rot = [0]

            def nbank():
                rot[0] = (rot[0] + 1) % 4
                return 4 + rot[0]

            def load_group(g):
                s_ = g % 2
                ck = 8 + g // 2
                cv = 10 + g // 2
                c0 = (g % 2) * 64
                srck = win[ck].rearrange("p (k n) -> p k n", k=16)[:, :, c0:c0 + 64]
                srcv = win[cv].rearrange("p (k n) -> p k n", k=16)[:, :, c0:c0 + 64]
                sc.dma("pool", lambda e: e.dma_start(out=wk2[s_][:, :, 0:64], in_=srck), chkv[s_], writes=[WKV[s_]])
                sc.dma("pool", lambda e: e.dma_start(out=wk2[s_][:, :, 64:128], in_=srck), chkv[s_], writes=[WKV[s_]])
                sc.dma("pool", lambda e: e.dma_start(out=wv[s_][:], in_=srcv), chkv[s_], writes=[WKV[s_]])

            def load_pair(c):
                s_ = c % 2
                sc.dma("pool", lambda e: e.dma_start(out=wq[s_][:].rearrange("p k n -> p (k n)"), in_=win[c]),
                       chq[s_], writes=[WQ[s_]])

            load_group(0)
            load_pair(0)
            for g in range(4):
                s_ = g % 2
                if g + 1 < 4:
                    load_group(g + 1)
                for tg in range(4):
                    bA = nbank()
                    bB = nbank()
                    fns = [(lambda kc, tg, bA, s_: lambda e: e.matmul(
                        ps[bA][:], lhsT=wk2[s_][:, kc, :], rhs=uT[:, kc, tg * 512:(tg + 1) * 512],
                        start=(kc == 0), stop=(kc == KC - 1)))(kc, tg, bA, s_) for kc in range(KC)]
                    sc.op("pe", fns, reads=[WKV[s_]] + tg_bufs(tg), writes=[PB[bA]])
                    i2 = tg % 2
                    qknorm(bA, bB, ct["blkones"], 1, sq[i2], SQ[i2], lnv[i2], LNV[i2], prm[:, 0:1],
                           [(0, 64, Klo[0:64, tg * 512:(tg + 1) * 512], [KB[tg]], 0, 512, None),
                            (64, 128, Khi[64:128, tg * 512:(tg + 1) * 512], [KB[tg]], 0, 512, None)])
                for half in range(2):
                    bV = nbank()
                    fns = []
                    for t8 in range(8):
                        t = half * 8 + t8
                        for kc in range(KC):
                            fns.append((lambda t, t8, kc, bV, s_: lambda e: e.matmul(
                                ps[bV][:, t8 * 64:(t8 + 1) * 64], lhsT=uT[:, kc, t * 128:(t + 1) * 128],
                                rhs=wv[s_][:, kc, :], start=(kc == 0), stop=(kc == KC - 1)))(t, t8, kc, bV, s_))
                    sc.op("pe", fns, reads=[WKV[s_]] + UT[half * 8:(half + 1) * 8], writes=[PB[bV]])
                    srcv = ps[bV][:].rearrange("p (t d) -> p t d", t=8)
                    ac((lambda half, srcv: lambda e: e.activation(
                        out=vlo[:, half * 8:(half + 1) * 8, 0:64], in_=srcv, func=AF.Copy))(half, srcv),
                       reads=[PB[bV]], writes=[VB[half]])
                    dv((lambda half, srcv: lambda e: e.tensor_copy(
                        out=vhi[:, half * 8:(half + 1) * 8, 64:128], in_=srcv))(half, srcv),
                       reads=[PB[bV]], writes=[VB[half]])
                for pp in range(2):
                    c = 2 * g + pp
                    sq_ = c % 2
                    if c + 1 < 8:
                        load_pair(c + 1)
                    for tg in range(4):
                        bA = nbank()
                        bB = nbank()
                        fns = [(lambda kc, tg, bA, sq_: lambda e: e.matmul(
                            ps[bA][:], lhsT=wq[sq_][:, kc, :], rhs=uT[:, kc, tg * 512:(tg + 1) * 512],
                            start=(kc == 0), stop=(kc == KC - 1)))(kc, tg, bA, sq_) for kc in range(KC)]
                        sc.op("pe", fns, reads=[WQ[sq_]] + tg_bufs(tg), writes=[PB[bA]])
                        i2 = tg % 2
                        qknorm(bA, bB, ct["blkones"], 1, sq[i2], SQ[i2], lnv[i2], LNV[i2], small[:, 0:1],
                               [(0, 128, qTa_[:, tg * 512:(tg + 1) * 512], [QB[tg]], 0, 512, None)])
                    sbank = {}

                    def a_S(m, c=c):
                        ncols = 256 if m < NT - 1 else 128
                        for e_ in range(2):
                            h = 2 * c + e_
                            bS = nbank()
                            sbank[(m, e_)] = bS
                            Kt = Klo if e_ == 0 else Khi
                            fns = [
                                (lambda Kt, m, ncols, bS: lambda e: e.matmul(
                                    ps[bS][:, :ncols], lhsT=Kt[:, m * 128:(m + 1) * 128],
                                    rhs=qTa_[:, m * 128:m * 128 + ncols], start=True, stop=False))(Kt, m, ncols, bS),
                                (lambda h, ncols, bS: lambda e: e.matmul(
                                    ps[bS][:, :ncols], lhsT=ct["ones3"][:], rhs=ct["atab"][:, h, :ncols],
                                    start=False, stop=False))(h, ncols, bS),
                                (lambda ncols, bS: lambda e: e.matmul(
                                    ps[bS][:, :ncols], lhsT=ct["ident"][:], rhs=ct["maskA"][:, :ncols],
                                    start=False, stop=True))(ncols, bS),
                            ]
                            qbs = [QB[m // 4]] + ([QB[(m + 1) // 4]] if m < NT - 1 else [])
                            sc.op("pe", fns, reads=[KB[m // 4], CB] + qbs, writes=[PB[bS]])

                    def a_E(m, c=c):
                        ncols = 256 if m < NT - 1 else 128
                        for e_ in range(2):
                            h = 2 * c + e_
                            bS = sbank[(m, e_)]
                            ac((lambda e_, m, ncols, bS, h: lambda e: e.activation(
                                out=PT[e_][m % 3][:, :ncols], in_=ps[bS][:, :ncols], func=AF.Exp,
                                bias=prm[:, 16 + h:17 + h], scale=1.0))(e_, m, ncols, bS, h),
                               reads=[PB[bS], PRM], writes=[PTB_[e_][m % 3]])

                    def a_PV(m, c=c):
                        qg = m // 4
                        bO = 2 * (qg % 2)
                        bD = bO + 1
                        for (bank, Lo, Hi, isv) in ((bO, vlo, vhi, True), (bD, ct["oneslo"], ct["oneshi"], False)):
                            fns = []
                            seq = []
                            for e_ in range(2):
                                W = Lo if e_ == 0 else Hi
                                if m > 0:
                                    seq.append((W, m - 1, PT[e_][(m - 1) % 3][:, 128:256]))
                                seq.append((W, m, PT[e_][m % 3][:, 0:128]))
                            for i_, (W, kt, rhs) in enumerate(seq):
                                lhs = W[:, kt, :] if isv else W[:]
                                fns.append((lambda lhs, rhs, i_, bank, m, n_=len(seq): lambda e: e.matmul(
                                    ps[bank][:, (m % 4) * 128:(m % 4 + 1) * 128], lhsT=lhs, rhs=rhs,
                                    start=(i_ == 0), stop=(i_ == n_ - 1)))(lhs, rhs, i_, bank, m))
                            rds = [PTB_[0][m % 3], PTB_[1][m % 3], VB[m // 8], CB]
                            if m > 0:
                                rds += [PTB_[0][(m - 1) % 3], PTB_[1][(m - 1) % 3], VB[(m - 1) // 8]]
                            sc.op("pe", fns, reads=rds, writes=[PB[bank]])
                        if m % 4 == 3:
                            dv((lambda bD, c: lambda e: e.tensor_scalar(
                                out=den[:], in0=ps[bD][:], scalar1=prm[:, 48 + c:49 + c], scalar2=None,
                                op0=ALU.add))(bD, c), reads=[PB[bD], PRM], writes=[DEN])
                            dv(lambda e: e.reciprocal(out=den[:], in_=den[:]), reads=[DEN], writes=[DEN])
                            dv((lambda bO, c, qg: lambda e: e.tensor_tensor(
                                out=oaT[:, c, qg * 512:(qg + 1) * 512], in0=ps[bO][:], in1=den[:],
                                op=ALU.mult))(bO, c, qg), reads=[PB[bO], DEN], writes=[OA[c][qg]])

                    a_S(0)
                    for m in range(NT):
                        if m + 1 < NT:
                            a_S(m + 1)
                        a_E(m)
                        a_PV(m)
            if DEBUG:
                chd2 = sc.chan("dbg2")
                for nm, t_, bufs in (("klo", Klo[:], KB), ("khi", Khi[:], KB), ("qTa", qTa_[:], QB),
                                     ("vlo", vlo[:].rearrange("p t d -> p (t d)"), VB),
                                     ("vhi", vhi[:].rearrange("p t d -> p (t d)"), VB)):
                    sc.dma("sp", (lambda nm, t_: lambda e: e.dma_start(out=dbg[nm], in_=t_))(nm, t_), chd2, reads=bufs)
                sc.dma("sp", lambda e: e.dma_start(out=dbg["pt0"], in_=PT[0][2][:]), chd2, reads=[PTB_[0][2]])
                sc.dma("sp", lambda e: e.dma_start(out=dbg["pt1"], in_=PT[1][2][:]), chd2, reads=[PTB_[1][2]])
                sc.dma("sp", lambda e: e.dma_start(out=dbg["prm"], in_=prm[:]), chd2, reads=[PRM])
            sc.barrier()

        with contextlib.ExitStack() as ph:
            alloc_consts(ph, [k for k in PH_CONSTS if PH_CONSTS[k] == 2])
            load_consts([k for k in PH_CONSTS if PH_CONSTS[k] == 2])
            wqb = [sb(ph, "b_wq%d" % i, [128, 16, 128], BF16) for i in range(2)]
            wkb = [sb(ph, "b_wk%d" % i, [128, 16, 128], BF16) for i in range(2)]
            wvb = [sb(ph, "b_wv%d" % i, [128, 16, 128], BF16) for i in range(2)]
            WBB = [Buf("b_w%d" % i) for i in range(2)]
            chw = [sc.chan("b_w%d" % i) for i in range(2)]
            kT = sb(ph, "b_kT", [128, S], BF16)
            qT = sb(ph, "b_qT", [128, S], BF16)
            vB = sb(ph, "b_v", [128, NT, 128], BF16)
            augb = sb(ph, "b_aug", [128, S], BF16)
            KB = [Buf("b_K%d" % i) for i in range(4)]
            QB = [Buf("b_Q%d" % i) for i in range(4)]
            VB = [Buf("b_V%d" % i) for i in range(4)]
            AG = Buf("b_aug")
            sq = [sb(ph, "b_sq%d" % i, [128, 512], BF16) for i in range(2)]
            lnv = [sb(ph, "b_ln%d" % i, [128, 512], F32) for i in range(2)]
            SQ = [Buf("b_sq%d" % i) for i in range(2)]
            LNV = [Buf("b_ln%d" % i) for i in range(2)]
            ksum = sb(ph, "b_ksum", [128, 8], F32)
            kmh = sb(ph, "b_kmh", [128, 8], BF16)
            kml = sb(ph, "b_kml", [128, 8], BF16)
            KS = Buf("b_ks")
            gm = sb(ph, "b_gm", [128, 128], F32)
            top8 = sb(ph, "b_top8", [128, 128], F32)
            gb_ = sb(ph, "b_gb", [128, 128], F32)
            tqs = sb(ph, "b_tqs", [128, 128], F32)
            comb = sb(ph, "b_comb", [128, 128], F32)
            cmb16 = sb(ph, "b_c16", [128, 16, 16], BF16)
            GT = Buf("b_gate")
            PTb = [sb(ph, "b_pt%d" % i, [128, 512], BF16) for i in range(3)]
            PTB_ = [Buf("b_pt%d" % i) for i in range(3)]
            rec = sb(ph, "b_rec", [128, 512], F32)
            REC = Buf("b_rec")
            sc.op("pool", lambda e: e.memset(augb[:], 0.0), writes=[AG])

            rot = [0]

            def nbank():
                rot[0] = (rot[0] + 1) % 4
                return 4 + rot[0]

            def load_head(h):
                s_ = h % 2
                for (t_, ci) in ((wqb, 12 + h), (wkb, 20 + h), (wvb, 28 + h)):
                    sc.dma("pool", (lambda t_, ci, s_: lambda e: e.dma_start(
                        out=t_[s_][:].rearrange("p k n -> p (k n)"), in_=win[ci]))(t_, ci, s_),
                        chw[s_], writes=[WBB[s_]])

            load_head(0)
            ptc = [0]
            for h in range(8):
                s_ = h % 2
                if h + 1 < 8:
                    load_head(h + 1)
                for tg in range(4):
                    bA = nbank()
                    bB = nbank()
                    fns = [(lambda kc, tg, bA, s_: lambda e: e.matmul(
                        ps[bA][:], lhsT=wkb[s_][:, kc, :], rhs=uT[:, kc, tg * 512:(tg + 1) * 512],
                        start=(kc == 0), stop=(kc == KC - 1)))(kc, tg, bA, s_) for kc in range(KC)]
                    sc.op("pe", fns, reads=[WBB[s_]] + tg_bufs(tg), writes=[PB[bA]])
                    i2 = tg % 2
                    qknorm(bA, bB, ct["ones128"], 2, sq[i2], SQ[i2], lnv[i2], LNV[i2], prm[:, 1:2],
                           [(0, 128, kT[:, tg * 512 + hh * 256: tg * 512 + (hh + 1) * 256], [KB[tg], KS],
                             hh * 256, (hh + 1) * 256, ksum[:, 2 * tg + hh: 2 * tg + hh + 1]) for hh in range(2)])
                dv(lambda e: e.tensor_scalar(out=kmh[:], in0=ksum[:], scalar1=1.0 / 256, scalar2=None, op0=ALU.mult),
                   reads=[KS], writes=[KS])
                dv(lambda e: e.scalar_tensor_tensor(out=kml[:], in0=ksum[:], scalar=1.0 / 256, in1=kmh[:],
                                                    op0=ALU.mult, op1=ALU.subtract), reads=[KS], writes=[KS])
                for tg in range(4):
                    bA = nbank()
                    bB = nbank()
                    fns = [(lambda kc, tg, bA, s_: lambda e: e.matmul(
                        ps[bA][:], lhsT=wqb[s_][:, kc, :], rhs=uT[:, kc, tg * 512:(tg + 1) * 512],
                        start=(kc == 0), stop=(kc == KC - 1)))(kc, tg, bA, s_) for kc in range(KC)]
                    sc.op("pe", fns, reads=[WBB[s_]] + tg_bufs(tg), writes=[PB[bA]])
                    i2 = tg % 2
                    qknorm(bA, bB, ct["ones128"], 2, sq[i2], SQ[i2], lnv[i2], LNV[i2], small[:, 2:3],
                           [(0, 128, qT[:, tg * 512:(tg + 1) * 512], [QB[tg]], 0, 512, None)])
                bG = nbank()
                fns = []
                for n in range(NT):
                    for ii, km in enumerate((kmh, kml)):
                        fns.append((lambda n, ii, km, bG: lambda e: e.matmul(
                            ps[bG][:, n * 8:(n + 1) * 8], lhsT=qT[:, n * 128:(n + 1) * 128], rhs=km[:],
                            start=(ii == 0), stop=(ii == 1)))(n, ii, km, bG))
                sc.op("pe", fns, reads=QB + [KS], writes=[PB[bG]])
                dv((lambda bG: lambda e: e.tensor_tensor(out=gm[:], in0=ps[bG][:, 0:128], in1=ct["cmask"][:],
                                                         op=ALU.add))(bG), reads=[PB[bG], CB], writes=[GT])
                for n in range(NT):
                    dv((lambda n: lambda e: e.max(out=top8[:, n * 8:(n + 1) * 8], in_=gm[:, n * 8:(n + 1) * 8]))(n),
                       reads=[GT], writes=[GT])
                for n in range(NT):
                    dv((lambda n: lambda e: e.tensor_scalar(
                        out=gb_[:, n * 8:(n + 1) * 8], in0=gm[:, n * 8:(n + 1) * 8],
                        scalar1=top8[:, n * 8 + 2:n * 8 + 3], scalar2=1.0, op0=ALU.is_ge, op1=ALU.subtract))(n),
                       reads=[GT], writes=[GT])
                dv(lambda e: e.tensor_tensor(out=gb_[:], in0=gb_[:], in1=ct["notown"][:], op=ALU.mult),
                   reads=[GT, CB], writes=[GT])
                dv((lambda h: lambda e: e.tensor_scalar(out=tqs[:], in0=ct["tqrel"][:], scalar1=float(-sB[h]),
                                                        scalar2=None, op0=ALU.mult))(h), reads=[CB, GT], writes=[GT])
                dv(lambda e: e.scalar_tensor_tensor(out=comb[:], in0=gb_[:], scalar=-NEG, in1=tqs[:],
                                                    op0=ALU.mult, op1=ALU.add), reads=[GT], writes=[GT])
                c3 = comb[:].rearrange("p (n b) -> p n b", n=16)
                dv(lambda e: e.tensor_copy(out=cmb16[:, :, 0:8], in_=c3), reads=[GT], writes=[GT])
                dv(lambda e: e.tensor_tensor(out=cmb16[:, :, 8:16], in0=c3, in1=cmb16[:, :, 0:8], op=ALU.subtract),
                   reads=[GT], writes=[GT])
                for qd in range(4):
                    bV = nbank()
                    fns = []
                    for t4 in range(4):
                        t = qd * 4 + t4
                        for kc in range(KC):
                            fns.append((lambda t, t4, kc, bV, s_: lambda e: e.matmul(
                                ps[bV][:, t4 * 128:(t4 + 1) * 128], lhsT=uT[:, kc, t * 128:(t + 1) * 128],
                                rhs=wvb[s_][:, kc, :], start=(kc == 0), stop=(kc == KC - 1)))(t, t4, kc, bV, s_))
                    sc.op("pe", fns, reads=[WBB[s_]] + UT[qd * 4:(qd + 1) * 4], writes=[PB[bV]])
                    srcv = ps[bV][:].rearrange("p (t d) -> p t d", t=4)
                    if qd % 2 == 0:
                        ac((lambda qd, srcv: lambda e: e.activation(
                            out=vB[:, qd * 4:(qd + 1) * 4, :], in_=srcv, func=AF.Copy))(qd, srcv),
                           reads=[PB[bV]], writes=[VB[qd]])
                    else:
                        dv((lambda qd, srcv: lambda e: e.tensor_copy(
                            out=vB[:, qd * 4:(qd + 1) * 4, :], in_=srcv))(qd, srcv),
                           reads=[PB[bV]], writes=[VB[qd]])
                for half in range(2):
                    bT = nbank()
                    pbf = ps[bT][:].bitcast(BF16)
                    fns = []
                    for jj in range(8):
                        n = half * 8 + jj
                        fns.append((lambda n, jj, pbf: lambda e: e.transpose(
                            out=pbf[0:16, jj * 128:(jj + 1) * 128], in_=cmb16[:, n, :],
                            identity=ct["ident"][:]))(n, jj, pbf))
                    sc.op("pe", fns, reads=[GT, CB], writes=[PB[bT]])
                    dv((lambda half, pbf: lambda e: e.tensor_copy(
                        out=augb[0:16, half * 1024:(half + 1) * 1024], in_=pbf[0:16, 0:1024]))(half, pbf),
                       reads=[PB[bT]], writes=[AG])
                steps = [(Q, kt) for Q in range(4) for kt in range(4 * Q + 4)]
                sbank = {}

                def geo(Q, kt):
                    q0 = max(kt * 128, Q * 512)
                    return q0, (Q + 1) * 512 - q0, q0 - Q * 512, kt * 128 >= Q * 512

                def b_S(i):
                    Q, kt = steps[i]
                    nb = kt // 2
                    q0, ncols, off, diag = geo(Q, kt)
                    bS = nbank()
                    sbank[i] = bS
                    fns = [
                        (lambda kt, q0, ncols, bS: lambda e: e.matmul(
                            ps[bS][:, :ncols], lhsT=kT[:, kt * 128:(kt + 1) * 128], rhs=qT[:, q0:q0 + ncols],
                            start=True, stop=False))(kt, q0, ncols, bS),
                        (lambda nb, q0, ncols, bS, diag: lambda e: e.matmul(
                            ps[bS][:, :ncols], lhsT=ct["enb"][:, nb, :], rhs=augb[:, q0:q0 + ncols],
                            start=False, stop=(not diag)))(nb, q0, ncols, bS, diag),
                    ]
                    if diag:
                        fns.append((lambda ncols, bS: lambda e: e.matmul(
                            ps[bS][:, :ncols], lhsT=ct["ident"][:], rhs=ct["maskB"][:, :ncols],
                            start=False, stop=True))(ncols, bS))
                    sc.op("pe", fns, reads=[KB[kt // 4], QB[Q], AG, CB], writes=[PB[bS]])

                def b_EPV(i, h=h):
                    Q, kt = steps[i]
                    half = kt % 2
                    q0, ncols, off, diag = geo(Q, kt)
                    bS = sbank[i]
                    bO = 2 * (Q % 2)
                    bD = bO + 1
                    nkt = 4 * Q + 4
                    sl = i % 3
                    ac((lambda sl, ncols, bS, h, half: lambda e: e.activation(
                        out=PTb[sl][:, :ncols], in_=ps[bS][:, :ncols], func=AF.Exp,
                        bias=prm[:, 32 + 2 * h + half:33 + 2 * h + half], scale=1.0))(sl, ncols, bS, h, half),
                       reads=[PB[bS], PRM], writes=[PTB_[sl]])
                    fns = [
                        (lambda kt, sl, ncols, off, bO, nkt: lambda e: e.matmul(
                            ps[bO][:, off:512], lhsT=vB[:, kt, :], rhs=PTb[sl][:, :ncols],
                            start=(kt == 0), stop=(kt == nkt - 1)))(kt, sl, ncols, off, bO, nkt),
                        (lambda kt, sl, ncols, off, bD, nkt: lambda e: e.matmul(
                            ps[bD][:, off:512], lhsT=ct["ones128"][:], rhs=PTb[sl][:, :ncols],
                            start=(kt == 0), stop=(kt == nkt - 1)))(kt, sl, ncols, off, bD, nkt),
                    ]
                    sc.op("pe", fns, reads=[PTB_[sl], VB[kt // 4], CB], writes=[PB[bO], PB[bD]])
                    if kt == nkt - 1:
                        dv((lambda bD: lambda e: e.reciprocal(out=rec[:], in_=ps[bD][:]))(bD),
                           reads=[PB[bD]], writes=[REC])
                        dv((lambda bO, h, Q: lambda e: e.tensor_tensor(
                            out=obT[:, h, Q * 512:(Q + 1) * 512], in0=ps[bO][:], in1=rec[:], op=ALU.mult))(bO, h, Q),
                           reads=[PB[bO], REC], writes=[OB[h][Q]])

                b_S(0)
                for i in range(len(steps)):
                    if i + 1 < len(steps):
                        b_S(i + 1)
                    b_EPV(i)
            sc.barrier()

        if DEBUG:
            sc.dma("sp", lambda e: e.dma_start(out=dbg["oaT"], in_=oaT[:].rearrange("p c t -> p (c t)")), chd,
                   reads=[b for r_ in OA for b in r_])
            sc.dma("sp", lambda e: e.dma_start(out=dbg["obT"], in_=obT[:].rearrange("p c t -> p (c t)")), chd,
                   reads=[b for r_ in OB for b in r_])

        OD = [[Buf("od%d_%d" % (t, c)) for c in range(16)] for t in range(NT)]

        with contextlib.ExitStack() as ph:
            mixT = sb(ph, "m_mix", [128, KC, 512], BF16)
            MX = [Buf("m_mx%d" % i) for i in range(KC)]
            wga = [sb(ph, "m_wga%d" % i, [128, 16, 128], BF16) for i in range(2)]
            wgb = [sb(ph, "m_wgb%d" % i, [128, 16, 128], BF16) for i in range(2)]
            wa = [sb(ph, "m_wa%d" % i, [128, 8, 128], BF16) for i in range(2)]
            wb = [sb(ph, "m_wb%d" % i, [128, 8, 128], BF16) for i in range(2)]
            WM = [Buf("m_w%d" % i) for i in range(2)]
            chm = [sc.chan("m_w%d" % i) for i in range(2)]
            wot = [sb(ph, "m_wo%d" % i, [128, 16, WOC], BF16) for i in range(2)]
            WO = [Buf("m_wo%d" % i) for i in range(2)]
            cho = [sc.chan("m_wo%d" % i) for i in range(2)]
            sga = sb(ph, "m_sga", [128, 512], F32)
            sgb = sb(ph, "m_sgb", [128, 512], F32)
            SG = [Buf("m_sga"), Buf("m_sgb")]
            NXR = 4
            xr = [sb(ph, "m_xr%d" % i, [128, WOC], F32) for i in range(NXR)]
            XR = [Buf("m_xr%d" % i) for i in range(NXR)]
            chx = [sc.chan("m_xr%d" % i) for i in range(NXR)]
            hs = [sb(ph, "m_hs%d" % i, [128, WOC], F32) for i in range(NXR)]
            HS = [Buf("m_hs%d" % i) for i in range(NXR)]
            chh = [sc.chan("m_hs%d" % i) for i in range(NXR)]
            store_chans = list(chh)

            jobs = []
            for tg in range(4):
                for oc in range(16):
                    jobs.append(("m", tg, oc))
                for cp in range(D // WOC):
                    jobs.append(("o", tg, cp))
            cntm = [0]
            cnto = [0]
            slot_of = {}
            for jb in jobs:
                if jb[0] == "m":
                    slot_of[jb] = cntm[0] % 2
                    cntm[0] += 1
                else:
                    slot_of[jb] = cnto[0] % 2
                    cnto[0] += 1

            def m_load(jb):
                s_ = slot_of[jb]
                if jb[0] == "m":
                    oc = jb[2]
                    for (t_, src) in ((wga, win[36 + oc]), (wgb, win[52 + oc]), (wa, wba[oc]), (wb, wbb[oc])):
                        sc.dma("pool", (lambda t_, src, s_: lambda e: e.dma_start(
                            out=t_[s_][:].rearrange("p k n -> p (k n)"), in_=src))(t_, src, s_),
                            chm[s_], writes=[WM[s_]])
                else:
                    cp = jb[2]
                    sc.dma("pool", (lambda cp, s_: lambda e: e.dma_start(
                        out=wot[s_][:].rearrange("p k n -> p (k n)"), in_=wo[cp]))(cp, s_), cho[s_], writes=[WO[s_]])

            bankset = [0]
            xc = [0]

            def m_compute(jb):
                s_ = slot_of[jb]
                tg = jb[1]
                tok = slice(tg * 512, (tg + 1) * 512)
                if jb[0] == "m":
                    oc = jb[2]
                    b0 = 4 * (bankset[0] % 2)
                    bankset[0] += 1
                    fns = []
                    for (bk, wt, src, nk) in ((b0, wga, uT, 16), (b0 + 1, wgb, uT, 16), (b0 + 2, wa, oaT, 8),
                                              (b0 + 3, wb, obT, 8)):
                        for kc in range(nk):
                            fns.append((lambda bk, wt, src, nk, kc: lambda e: e.matmul(
                                ps[bk][:], lhsT=wt[s_][:, kc, :], rhs=src[:, kc, tok],
                                start=(kc == 0), stop=(kc == nk - 1)))(bk, wt, src, nk, kc))
                    rds = [WM[s_]] + tg_bufs(tg) + [OA[c][tg] for c in range(8)] + [OB[c][tg] for c in range(8)]
                    sc.op("pe", fns, reads=rds, writes=[PB[b0], PB[b0 + 1], PB[b0 + 2], PB[b0 + 3]])
                    ac(lambda e: e.activation(out=sga[:], in_=ps[b0][:], func=AF.Sigmoid,
                                              bias=small[:, 12 + oc:13 + oc], scale=1.0),
                       reads=[PB[b0], CB], writes=[SG[0]])
                    ac(lambda e: e.activation(out=sgb[:], in_=ps[b0 + 1][:], func=AF.Sigmoid,
                                              bias=small[:, 28 + oc:29 + oc], scale=1.0),
                       reads=[PB[b0 + 1], CB], writes=[SG[1]])
                    dv(lambda e: e.tensor_tensor(out=sga[:], in0=sga[:], in1=ps[b0 + 2][:], op=ALU.mult),
                       reads=[SG[0], PB[b0 + 2]], writes=[SG[0]])
                    dv(lambda e: e.tensor_tensor(out=sgb[:], in0=sgb[:], in1=ps[b0 + 3][:], op=ALU.mult),
                       reads=[SG[1], PB[b0 + 3]], writes=[SG[1]])
                    dv(lambda e: e.tensor_tensor(out=mixT[:, oc, :], in0=sga[:], in1=sgb[:], op=ALU.add),
                       reads=SG, writes=[MX[oc]])
                else:
                    cp = jb[2]
                    cols = slice(cp * WOC, (cp + 1) * WOC)
                    for tt in range(4):
                        t = tg * 4 + tt
                        i2 = xc[0] % NXR
                        xc[0] += 1
                        rows = slice(t * 128, (t + 1) * 128)
                        sc.dma("act", (lambda i2, rows: lambda e: e.dma_start(out=xr[i2][:], in_=x[rows, cols]))(i2, rows),
                               chx[i2], writes=[XR[i2]])
                        bk = (xc[0]) % 8
                        fns = [(lambda kc, tt, bk: lambda e: e.matmul(
                            ps[bk][:, :WOC], lhsT=mixT[:, kc, tt * 128:(tt + 1) * 128], rhs=wot[s_][:, kc, :],
                            start=(kc == 0), stop=(kc == KC - 1)))(kc, tt, bk) for kc in range(KC)]
                        sc.op("pe", fns, reads=MX + [WO[s_]], writes=[PB[bk]])
                        dv((lambda i2, bk: lambda e: e.tensor_tensor(out=hs[i2][:], in0=ps[bk][:, :WOC], in1=xr[i2][:],
                                                                     op=ALU.add))(i2, bk),
                           reads=[PB[bk], XR[i2]], writes=[HS[i2]])
                        ods = [OD[t][cc] for cc in range(cp * WOC // 128, (cp + 1) * WOC // 128)]
                        sc.dma("sp", (lambda i2, rows: lambda e: e.dma_start(out=out[rows, cols], in_=hs[i2][:]))(i2, rows),
                               chh[i2], reads=[HS[i2]], writes=ods)
                        if DEBUG:
                            sc.dma("sp", (lambda i2, rows: lambda e: e.dma_start(out=dbg["h1"][rows, cols], in_=hs[i2][:]))(i2, rows),
                                   chh[i2], reads=[HS[i2]])

            m_load(jobs[0])
            for i, jb in enumerate(jobs):
                if i + 1 < len(jobs):
                    m_load(jobs[i + 1])
                m_compute(jb)
            sc.barrier()

        pers.close()

        with contextlib.ExitStack() as ph:
            T4 = 1024
            u2T = sb(ph, "f_u2T", [128, KC, T4], BF16)
            U2 = [Buf("f_u2%d" % i) for i in range(T4 // 128)]
            actT = sb(ph, "f_act", [128, FC, T4], BF16)
            ACT_ = [[Buf("f_act%d_%d" % (oc, i)) for i in range(2)] for oc in range(FC)]
            wgt = [sb(ph, "f_wg%d" % i, [128, 16, 128], BF16) for i in range(2)]
            wut = [sb(ph, "f_wu%d" % i, [128, 16, 128], BF16) for i in range(2)]
            WGU = [Buf("f_wgu%d" % i) for i in range(2)]
            chgu = [sc.chan("f_gu%d" % i) for i in range(2)]
            wdt = [sb(ph, "f_wd%d" % i, [128, FC, 128], BF16) for i in range(2)]
            WD = [Buf("f_wd%d" % i) for i in range(2)]
            chd_ = [sc.chan("f_wd%d" % i) for i in range(2)]
            sg = [sb(ph, "f_sg%d" % i, [128, 512], F32) for i in range(2)]
            SGf = [Buf("f_sg%d" % i) for i in range(2)]
            yT = [sb(ph, "f_yT%d" % i, [128, 512], F32) for i in range(2)]
            YT = [Buf("f_yT%d" % i) for i in range(2)]
            h1r = [sb(ph, "f_h1r%d" % i, [128, 4, 128], F32) for i in range(2)]
            H1R = [Buf("f_h1r%d" % i) for i in range(2)]
            chr_ = [sc.chan("f_h1r%d" % i) for i in range(2)]
            yo = [sb(ph, "f_yo%d" % i, [128, 4, 128], F32) for i in range(2)]
            YO = [Buf("f_yo%d" % i) for i in range(2)]
            chy = [sc.chan("f_yo%d" % i) for i in range(2)]
            store_chans2 = list(chy)
            outv = out.rearrange("(t p) f -> p t f", p=128)

            with contextlib.ExitStack() as ph_n:
                pass
            for hf in range(2):
                t0 = hf * (T4 // 128)
                with contextlib.ExitStack() as phn:
                    pass
                if hf == 0:
                    nstack = ph
                    n_xt = [sb(ph, "f_xt%d" % i, [128, D], F32) for i in range(2)]
                    n_xn1 = sb(ph, "f_xn0", [128, D], BF16)
                    n_xn = [n_xn1, n_xn1]
                    n_gb = sb(ph, "f_gb", [128, D], F32)
                    n_st = sb(ph, "f_st", [128, 3 * NT], F32)
                    NXT = [Buf("f_xt%d" % i) for i in range(2)]
                    NXN1 = Buf("f_xn0")
                    NXN = [NXN1, NXN1]
                    NGB = Buf("f_gb")
                    NST = [Buf("f_st%d" % i) for i in range(NT)]
                    chnx = [sc.chan("f_x%d" % i) for i in range(2)]
                    chng = sc.chan("f_g")
                    sc.dma("sp", lambda e: e.dma_start(out=n_gb[:], in_=g2b_d), chng, writes=[NGB])
                for tl in range(T4 // 128):
                    t = t0 + tl
                    s_ = t % 2
                    sc.dma("sp", (lambda s_, t: lambda e: e.dma_start(out=n_xt[s_][:], in_=out[t * 128:(t + 1) * 128, :]))(s_, t),
                           chnx[s_], reads=OD[t], writes=[NXT[s_]])
                    ac((lambda s_, t: lambda e: e.activation(out=n_xn[s_][:], in_=n_xt[s_][:], func=AF.Square,
                                                             accum_out=n_st[:, 3 * t:3 * t + 1]))(s_, t),
                       reads=[NXT[s_]], writes=[NXN[s_], NST[t]])
                    ac((lambda t: lambda e: e.activation(out=n_st[:, 3 * t + 1:3 * t + 2], in_=n_st[:, 3 * t:3 * t + 1],
                                                         func=AF.Ln, bias=ct_eps[:, 0:1], scale=1.0 / D))(t),
                       reads=[NST[t], PRM], writes=[NST[t]])
                    ac((lambda t: lambda e: e.activation(out=n_st[:, 3 * t + 2:3 * t + 3],
                                                         in_=n_st[:, 3 * t + 1:3 * t + 2], func=AF.Exp, scale=-0.5))(t),
                       reads=[NST[t]], writes=[NST[t]])
                    dv((lambda s_, t: lambda e: e.scalar_tensor_tensor(
                        out=n_xn[s_][:], in0=n_xt[s_][:], scalar=n_st[:, 3 * t + 2:3 * t + 3], in1=n_gb[:],
                        op0=ALU.mult, op1=ALU.mult))(s_, t), reads=[NXT[s_], NST[t], NGB], writes=[NXN[s_]])
                    for half in range(2):
                        bk = (2 * t + half) % 8
                        pbf = ps[bk][:].bitcast(BF16)
                        fns = []
                        for jj in range(8):
                            c = half * 8 + jj
                            fns.append((lambda s_, c, jj, pbf: lambda e: e.transpose(
                                out=pbf[:, jj * 128:(jj + 1) * 128], in_=n_xn[s_][:, c * 128:(c + 1) * 128],
                                identity=ct["ident"][:]))(s_, c, jj, pbf))
                        sc.op("pe", fns, reads=[NXN[s_], CB], writes=[PB[bk]])
                        dstv = u2T[:, half * 8:(half + 1) * 8, tl * 128:(tl + 1) * 128]
                        srcv = pbf[:, 0:1024].rearrange("p (c t) -> p c t", c=8)
                        if half == 0:
                            ac((lambda dstv, srcv: lambda e: e.activation(out=dstv, in_=srcv, func=AF.Copy))(dstv, srcv),
                               reads=[PB[bk]], writes=[U2[tl]])
                        else:
                            dv((lambda dstv, srcv: lambda e: e.tensor_copy(out=dstv, in_=srcv))(dstv, srcv),
                               reads=[PB[bk]], writes=[U2[tl]])

                def gu_load(oc):
                    s_ = oc % 2
                    for (t_, src) in ((wgt, wg[oc]), (wut, wu[oc])):
                        sc.dma("pool", (lambda t_, src, s_: lambda e: e.dma_start(
                            out=t_[s_][:].rearrange("p k n -> p (k n)"), in_=src))(t_, src, s_),
                            chgu[s_], writes=[WGU[s_]])

                def wd_load(oc):
                    s_ = oc % 2
                    sc.dma("pool", (lambda oc, s_: lambda e: e.dma_start(
                        out=wdt[s_][:].rearrange("p k n -> p (k n)"), in_=wd[oc]))(oc, s_), chd_[s_], writes=[WD[s_]])

                gu_load(0)
                bc = [0]
                for oc in range(FC):
                    s_ = oc % 2
                    if oc + 1 < FC:
                        gu_load(oc + 1)
                    elif True:
                        wd_load(0)
                    for t2 in range(2):
                        bG_ = 2 * (bc[0] % 4)
                        bU_ = bG_ + 1
                        i2 = bc[0] % 2
                        bc[0] += 1
                        tok = slice(t2 * 512, (t2 + 1) * 512)
                        fns = []
                        for (bk, wt) in ((bG_, wgt), (bU_, wut)):
                            for kc in range(KC):
                                fns.append((lambda bk, wt, kc, s_, tok: lambda e: e.matmul(
                                    ps[bk][:], lhsT=wt[s_][:, kc, :], rhs=u2T[:, kc, tok],
                                    start=(kc == 0), stop=(kc == KC - 1)))(bk, wt, kc, s_, tok))
                        sc.op("pe", fns, reads=[WGU[s_]] + U2[t2 * 4:(t2 + 1) * 4], writes=[PB[bG_], PB[bU_]])
                        ac((lambda i2, bG_: lambda e: e.activation(out=sg[i2][:], in_=ps[bG_][:], func=AF.Silu))(i2, bG_),
                           reads=[PB[bG_]], writes=[SGf[i2]])
                        dv((lambda i2, bU_, oc, tok: lambda e: e.tensor_tensor(
                            out=actT[:, oc, tok], in0=sg[i2][:], in1=ps[bU_][:], op=ALU.mult))(i2, bU_, oc, tok),
                           reads=[SGf[i2], PB[bU_]], writes=[ACT_[oc][t2]])
                steps_c = [(oc, t2) for oc in range(16) for t2 in range(2)]
                cinfo = {}

                def c_mm(j, t0=t0):
                    oc, t2 = steps_c[j]
                    s_ = oc % 2
                    if t2 == 0 and oc + 1 < 16:
                        wd_load(oc + 1)
                    i2 = bc[0] % 2
                    bY = 2 * (bc[0] % 4)
                    bT = bY + 1
                    bc[0] += 1
                    tok = slice(t2 * 512, (t2 + 1) * 512)
                    tts = slice(t0 + t2 * 4, t0 + t2 * 4 + 4)
                    ods = [OD[t][oc] for t in range(t0 + t2 * 4, t0 + t2 * 4 + 4)]
                    cinfo[j] = (i2, bY, bT, tts, ods, oc)
                    sc.dma("act", (lambda i2, tts, oc: lambda e: e.dma_start(
                        out=h1r[i2][:], in_=outv[:, tts, oc * 128:(oc + 1) * 128]))(i2, tts, oc),
                        chr_[i2], reads=ods, writes=[H1R[i2]])
                    fns = [(lambda kc, s_, tok, bY, oc: lambda e: e.matmul(
                        ps[bY][:], lhsT=wdt[s_][:, kc, :], rhs=actT[:, kc, tok],
                        start=(kc == 0), stop=(kc == FC - 1)))(kc, s_, tok, bY, oc) for kc in range(FC)]
                    sc.op("pe", fns, reads=[WD[s_]] + [ACT_[k][t2] for k in range(FC)], writes=[PB[bY]])

                def c_tail(j):
                    i2, bY, bT, tts, ods, oc = cinfo[j]
                    ac((lambda i2, bY: lambda e: e.activation(out=yT[i2][:], in_=ps[bY][:], func=AF.Copy))(i2, bY),
                       reads=[PB[bY]], writes=[YT[i2]])
                    fns = [(lambda jj, i2, bT: lambda e: e.transpose(
                        out=ps[bT][:, jj * 128:(jj + 1) * 128], in_=yT[i2][:, jj * 128:(jj + 1) * 128],
                        identity=ct["identf"][:]))(jj, i2, bT) for jj in range(4)]
                    sc.op("pe", fns, reads=[YT[i2], CB], writes=[PB[bT]])
                    dv((lambda i2, bT: lambda e: e.tensor_tensor(
                        out=yo[i2][:], in0=ps[bT][:].rearrange("p (t f) -> p t f", t=4), in1=h1r[i2][:],
                        op=ALU.add))(i2, bT), reads=[PB[bT], H1R[i2]], writes=[YO[i2]])
                    sc.dma("sp", (lambda i2, tts, oc: lambda e: e.dma_start(
                        out=outv[:, tts, oc * 128:(oc + 1) * 128], in_=yo[i2][:]))(i2, tts, oc),
                        chy[i2], reads=[YO[i2]], writes=ods)

                c_mm(0)
                for j in range(len(steps_c)):
                    if j + 1 < len(steps_c):
                        c_mm(j + 1)
                    c_tail(j)
            sc.barrier()

        with nc.Block() as block:
            @block.tensor
            def _(eng):
                sc.replay("pe", eng)

            @block.scalar
            def _(eng):
                sc.replay("act", eng)

            @block.vector
            def _(eng):
                sc.replay("dve", eng)

            @block.gpsimd
            def _(eng):
                sc.replay("pool", eng)

            @block.sync
            def _(eng):
                sc.replay("sp", eng)
    return nc


def _tile_w(w, kc, ncols, width):
    return np.ascontiguousarray(
        w.reshape(kc, 128, ncols, width).transpose(2, 1, 0, 3)).reshape(ncols, 128, kc * width)


_CACHE = {}


def kernel(x, norm1_g, w_in, b_gate, q_norm_a, k_norm_a, sinks_a, q_norm_b, k_norm_b,
           w_branch_a, w_branch_b, w_o, norm2_g, w_ffn_gate, w_ffn_up, w_ffn_down):
    f32 = np.float32
    x = np.asarray(x, f32)
    consts = make_consts()
    if "nc" not in _CACHE:
        _CACHE["nc"] = build_nc(consts)
    nc = _CACHE["nc"]

    shared = {}
    shared["win_t"] = _tile_w(np.asarray(w_in, f32)[0], 16, 68, 128)
    shared["wba_t"] = _tile_w(np.asarray(w_branch_a, f32)[0], 8, 16, 128)
    shared["wbb_t"] = _tile_w(np.asarray(w_branch_b, f32)[0], 8, 16, 128)
    shared["wo_t"] = _tile_w(np.asarray(w_o, f32)[0], 16, D // WOC, WOC)
    shared["wg_t"] = _tile_w(np.asarray(w_ffn_gate, f32)[0], 16, FC, 128)
    shared["wu_t"] = _tile_w(np.asarray(w_ffn_up, f32)[0], 16, FC, 128)
    shared["wd_t"] = _tile_w(np.asarray(w_ffn_down, f32)[0], FC, 16, 128)
    shared["g1b"] = np.ascontiguousarray(np.broadcast_to(np.asarray(norm1_g, f32)[0][None, :], (128, D)))
    shared["g2b"] = np.ascontiguousarray(np.broadcast_to(np.asarray(norm2_g, f32)[0][None, :], (128, D)))
    small = np.zeros((128, 64), f32)
    qa = np.asarray(q_norm_a, f32)[0]
    ka = np.asarray(k_norm_a, f32)[0]
    qb = np.asarray(q_norm_b, f32)[0]
    kb = np.asarray(k_norm_b, f32)[0]
    small[:64, 0] = qa
    small[64:, 0] = qa
    small[:64, 1] = ka
    small[64:, 1] = ka
    small[:, 2] = qb
    small[:, 3] = kb
    sk = np.asarray(sinks_a, f32)[0]
    for c in range(8):
        small[:64, 4 + c] = sk[2 * c]
        small[64:, 4 + c] = sk[2 * c + 1]
    small[:, 12:44] = np.asarray(b_gate, f32)[0].reshape(32, 128).T
    shared["small"] = small
    gbro = np.zeros((128, 384), f32)
    gbro[:, 0:64] = qa[None, :]
    gbro[:, 64:128] = ka[None, :]
    gbro[:, 128:256] = qb[None, :]
    gbro[:, 256:384] = kb[None, :]
    shared["gbro"] = gbro
    for k, v in consts.items():
        shared["c_" + k] = v

    in_maps = []
    for b in range(8):
        m = dict(shared)
        m["x"] = np.ascontiguousarray(x[b])
        in_maps.append(m)
    res = run_bass_kernel_spmd(nc, in_maps, core_ids=list(range(8)))
    _CACHE["last"] = res
    outp = np.stack([np.asarray(r["out"], f32) for r in res.results], axis=0)
    return outp
```
